# Optimizing a Trainium2 kernel written in Bass

```python
import jax, jax.numpy as jnp
from jax import lax
import numpy as np

D_MODEL = 1024
BATCH = 8
SEQ = 8192
DEPTH = 1

N_MEM = 256
EPS = 1e-6
NEG = -1e30
ROPE_THETA = 10000.0
MOBA_HEADS = 8
MOBA_HD = 64
MOBA_WIDTH = MOBA_HEADS * MOBA_HD
MOBA_BLOCK = 256
MOBA_TOPK = 3
Q_CHUNK = 128
GMLP_GROUPS = 8
GMLP_GD = 64
GMLP_WIDTH = GMLP_GROUPS * GMLP_GD
GMLP_CHUNK = 128
N_BRANCH = 2
IN_COLS = 3 * MOBA_WIDTH + 2 * GMLP_WIDTH + N_BRANCH * D_MODEL
XA_HEADS = 4
XA_HD = 128
XA_WIDTH = XA_HEADS * XA_HD
PEER_HEADS = 8
PEER_NKEYS = 128
PEER_N = PEER_NKEYS * PEER_NKEYS
PEER_DQ = 256
PEER_TOPK = 16
PEER_TOK_CHUNK = 128

kernel_name = "moba_gmlp_gated_peer_hybrid"


def rmsnorm(x, g):
    xf = x.astype(jnp.float32)
    y = xf * lax.rsqrt(jnp.mean(xf * xf, axis=-1, keepdims=True) + EPS)
    return (y * g.astype(jnp.float32)).astype(x.dtype)


def rope(x):
    S, Dh = x.shape[1], x.shape[3]
    half = Dh // 2
    freqs = ROPE_THETA ** (-jnp.arange(half, dtype=jnp.float32) / half)
    ang = jnp.arange(S, dtype=jnp.float32)[:, None] * freqs[None, :]
    c = jnp.cos(ang)[None, :, None, :]
    s = jnp.sin(ang)[None, :, None, :]
    xf = x.astype(jnp.float32)
    x1, x2 = xf[..., :half], xf[..., half:]
    return jnp.concatenate([x1 * c - x2 * s, x2 * c + x1 * s], axis=-1).astype(x.dtype)


def moba_attention(q, k, v):
    B, S, H, Dh = q.shape
    nb = -(-S // MOBA_BLOCK)
    pad = nb * MOBA_BLOCK - S
    nq = S // Q_CHUNK
    top = min(MOBA_TOPK, nb)
    kp = jnp.pad(k, ((0, 0), (0, pad), (0, 0), (0, 0)))
    vp = jnp.pad(v, ((0, 0), (0, pad), (0, 0), (0, 0)))
    kb = kp.reshape(B, nb, MOBA_BLOCK, H, Dh).transpose(0, 3, 1, 2, 4)
    vb = vp.reshape(B, nb, MOBA_BLOCK, H, Dh).transpose(0, 3, 1, 2, 4)
    k_mean = jnp.mean(kb.astype(jnp.float32), axis=3)
    q_blk = jnp.arange(S) // MOBA_BLOCK
    past = jnp.arange(nb)[None, :] < q_blk[:, None]
    gate = jnp.einsum("bshd,bhnd->bhsn", q.astype(jnp.float32), k_mean)
    gate = jnp.where(past[None, None], gate, NEG)
    _, sel = lax.top_k(gate, top)
    q_c = q.reshape(B, nq, Q_CHUNK, H, Dh).transpose(0, 1, 3, 2, 4).reshape(B * nq, H, Q_CHUNK, Dh)
    sel_c = sel.reshape(B, H, nq, Q_CHUNK, top).transpose(0, 2, 1, 3, 4).reshape(B * nq, H, Q_CHUNK, top)
    scale = Dh ** -0.5
    h_idx = jnp.arange(H)[:, None, None]

    def query_block(args):
        i, qc, si = args
        b = i // nq
        c = i % nq
        kb_b = kb[b]
        vb_b = vb[b]
        pos = c * Q_CHUNK + jnp.arange(Q_CHUNK)
        valid = si < (pos // MOBA_BLOCK)[None, :, None]
        k_sel = kb_b[h_idx, si]
        v_sel = vb_b[h_idx, si]
        s_sel = jnp.einsum("hqd,hqnld->hqnl", qc, k_sel).astype(jnp.float32) * scale
        s_sel = jnp.where(valid[..., None], s_sel, NEG).reshape(H, Q_CHUNK, top * MOBA_BLOCK)
        own = (c * Q_CHUNK) // MOBA_BLOCK
        k_own = lax.dynamic_index_in_dim(kb_b, own, axis=1, keepdims=False)
        v_own = lax.dynamic_index_in_dim(vb_b, own, axis=1, keepdims=False)
        s_own = jnp.einsum("hqd,hld->hql", qc, k_own).astype(jnp.float32) * scale
        k_pos = own * MOBA_BLOCK + jnp.arange(MOBA_BLOCK)
        s_own = jnp.where((k_pos[None, :] <= pos[:, None])[None], s_own, NEG)
        p = jax.nn.softmax(jnp.concatenate([s_sel, s_own], axis=-1), axis=-1).astype(qc.dtype)
        p_sel = p[..., :top * MOBA_BLOCK].reshape(H, Q_CHUNK, top, MOBA_BLOCK)
        p_own = p[..., top * MOBA_BLOCK:]
        return (jnp.einsum("hqnl,hqnld->hqd", p_sel, v_sel)
                + jnp.einsum("hql,hld->hqd", p_own, v_own))

    out = lax.map(query_block, (jnp.arange(B * nq), q_c, sel_c))
    return out.reshape(B, nq, H, Q_CHUNK, Dh).transpose(0, 1, 3, 2, 4).reshape(B, S, H * Dh)


def gmlp_spatial_gating(u, v, norm_g, w_s, b_s):
    B, S, _ = u.shape
    nc = S // GMLP_CHUNK
    u = jax.nn.gelu(u)
    v = rmsnorm(jax.nn.gelu(v), norm_g)
    vc = v.reshape(B, nc, GMLP_CHUNK, GMLP_GROUPS, GMLP_GD)
    causal = jnp.tril(jnp.ones((GMLP_CHUNK, GMLP_CHUNK), dtype=bool))
    wm = jnp.where(causal[None], w_s, 0.0).astype(v.dtype)
    z = jnp.einsum("gts,bcsgd->bctgd", wm, vc) + b_s.T.astype(v.dtype)[None, None, :, :, None]
    return u * z.reshape(B, S, GMLP_WIDTH)


def memory_cross_attention(h, m, w_q, w_kv, w_o):
    B, S, _ = h.shape
    M = m.shape[1]
    q = (h @ w_q).reshape(B, S, XA_HEADS, XA_HD)
    k, v = jnp.split(m @ w_kv, 2, axis=-1)
    k = k.reshape(B, M, XA_HEADS, XA_HD)
    v = v.reshape(B, M, XA_HEADS, XA_HD)
    s = jnp.einsum("bshd,bmhd->bhsm", q, k).astype(jnp.float32) * (XA_HD ** -0.5)
    p = jax.nn.softmax(s, axis=-1).astype(h.dtype)
    o = jnp.einsum("bhsm,bmhd->bshd", p, v).reshape(B, S, XA_WIDTH)
    return o @ w_o


def peer_ffn(h, w_q, subkeys, w_down, w_up):
    B, S, D = h.shape
    n_chunks = (B * S) // PEER_TOK_CHUNK
    half = PEER_DQ // 2
    kk = PEER_TOPK * PEER_TOPK

    def token_chunk(xc):
        q = (xc @ w_q).reshape(PEER_TOK_CHUNK, PEER_HEADS, 2, half).astype(jnp.float32)
        s = jnp.einsum("thpd,hpnd->thpn", q, subkeys.astype(jnp.float32))
        s1, i1 = lax.top_k(s[:, :, 0], PEER_TOPK)
        s2, i2 = lax.top_k(s[:, :, 1], PEER_TOPK)
        cand_s = (s1[..., :, None] + s2[..., None, :]).reshape(PEER_TOK_CHUNK, PEER_HEADS, kk)
        cand_i = (i1[..., :, None] * PEER_NKEYS + i2[..., None, :]).reshape(PEER_TOK_CHUNK, PEER_HEADS, kk)
        top_s, top_pos = lax.top_k(cand_s, PEER_TOPK)
        e = jnp.take_along_axis(cand_i, top_pos, axis=-1)
        g = jax.nn.softmax(top_s, axis=-1).astype(xc.dtype)
        a = jax.nn.gelu(jnp.einsum("td,thkd->thk", xc, w_down[e]))
        return jnp.einsum("thk,thkd->td", g * a, w_up[e])

    y = lax.map(token_chunk, h.reshape(n_chunks, PEER_TOK_CHUNK, D))
    return y.reshape(B, S, D)


def setup_inputs(seed: int = 0) -> dict:
    key = jax.random.key(seed)
    ks = jax.random.split(key, 24)

    def nrm(k, shape, scale):
        return jax.random.normal(k, shape, jnp.float32) * scale

    def gain(k, shape):
        return 1.0 + 0.01 * jax.random.normal(k, shape, jnp.float32)

    return {
        "x": nrm(ks[0], (BATCH, SEQ, D_MODEL), 1.0),
        "mem": nrm(ks[1], (BATCH, N_MEM, D_MODEL), 1.0),
        "norm_mix_g": gain(ks[2], (DEPTH, D_MODEL)),
        "w_in": nrm(ks[3], (DEPTH, D_MODEL, IN_COLS), D_MODEL ** -0.5),
        "moba_w_proj": nrm(ks[4], (DEPTH, MOBA_WIDTH, D_MODEL), MOBA_WIDTH ** -0.5),
        "gmlp_norm_g": gain(ks[5], (DEPTH, GMLP_WIDTH)),
        "gmlp_w_s": nrm(ks[6], (DEPTH, GMLP_GROUPS, GMLP_CHUNK, GMLP_CHUNK), GMLP_CHUNK ** -0.5),
        "gmlp_b_s": 1.0 + 0.1 * jax.random.normal(ks[7], (DEPTH, GMLP_GROUPS, GMLP_CHUNK), jnp.float32),
        "gmlp_w_proj": nrm(ks[8], (DEPTH, GMLP_WIDTH, D_MODEL), GMLP_WIDTH ** -0.5),
        "w_out": nrm(ks[9], (DEPTH, D_MODEL, D_MODEL), D_MODEL ** -0.5),
        "norm_xa_g": gain(ks[10], (DEPTH, D_MODEL)),
        "norm_mem_g": gain(ks[11], (DEPTH, D_MODEL)),
        "xa_w_q": nrm(ks[12], (DEPTH, D_MODEL, XA_WIDTH), D_MODEL ** -0.5),
        "xa_w_kv": nrm(ks[13], (DEPTH, D_MODEL, 2 * XA_WIDTH), D_MODEL ** -0.5),
        "xa_w_o": nrm(ks[14], (DEPTH, XA_WIDTH, D_MODEL), XA_WIDTH ** -0.5),
        "norm_ffn_g": gain(ks[15], (DEPTH, D_MODEL)),
        "peer_w_q": nrm(ks[16], (DEPTH, D_MODEL, PEER_HEADS * PEER_DQ), D_MODEL ** -0.5),
        "peer_subkeys": nrm(ks[17], (DEPTH, PEER_HEADS, 2, PEER_NKEYS, PEER_DQ // 2), (PEER_DQ // 2) ** -0.5),
        "peer_w_down": nrm(ks[18], (DEPTH, PEER_N, D_MODEL), D_MODEL ** -0.5),
        "peer_w_up": nrm(ks[19], (DEPTH, PEER_N, D_MODEL), 0.3),
        "final_g": gain(ks[20], (D_MODEL,)),
    }


def reference(x, mem, norm_mix_g, w_in, moba_w_proj, gmlp_norm_g, gmlp_w_s, gmlp_b_s,
              gmlp_w_proj, w_out, norm_xa_g, norm_mem_g, xa_w_q, xa_w_kv, xa_w_o,
              norm_ffn_g, peer_w_q, peer_subkeys, peer_w_down, peer_w_up, final_g):
    B, S, _ = x.shape
    splits = list(np.cumsum([MOBA_WIDTH, MOBA_WIDTH, MOBA_WIDTH, GMLP_WIDTH, GMLP_WIDTH, D_MODEL]))
    for l in range(DEPTH):
        h = rmsnorm(x, norm_mix_g[l])
        proj = h @ w_in[l]
        q, k, v, gu, gv, gate_a, gate_b = jnp.split(proj, splits, axis=-1)
        q = rope(q.reshape(B, S, MOBA_HEADS, MOBA_HD))
        k = rope(k.reshape(B, S, MOBA_HEADS, MOBA_HD))
        v = v.reshape(B, S, MOBA_HEADS, MOBA_HD)
        y_a = moba_attention(q, k, v) @ moba_w_proj[l]
        y_b = gmlp_spatial_gating(gu, gv, gmlp_norm_g[l], gmlp_w_s[l], gmlp_b_s[l]) @ gmlp_w_proj[l]
        merged = jax.nn.sigmoid(gate_a) * y_a + jax.nn.sigmoid(gate_b) * y_b
        x = x + merged @ w_out[l]
        h = rmsnorm(x, norm_xa_g[l])
        m = rmsnorm(mem, norm_mem_g[l])
        x = x + memory_cross_attention(h, m, xa_w_q[l], xa_w_kv[l], xa_w_o[l])
        h = rmsnorm(x, norm_ffn_g[l])
        x = x + peer_ffn(h, peer_w_q[l], peer_subkeys[l], peer_w_down[l], peer_w_up[l])
    return rmsnorm(x, final_g)
```

```python
import contextlib
import os
import numpy as np
import ml_dtypes
import concourse.bass as bass
import concourse.mybir as mybir
from concourse.bass_utils import run_bass_kernel_spmd

F32 = mybir.dt.float32
BF16 = mybir.dt.bfloat16
U32 = mybir.dt.uint32
AF = mybir.ActivationFunctionType
ALU = mybir.AluOpType
AX = mybir.AxisListType
D = 1024
KC = 8
EPS = 1e-6
GELU = AF.Gelu_apprx_tanh
NEGM = -30000.0


class Buf:
    __slots__ = ("w", "r", "sem", "dc", "name")

    def __init__(self, name=""):
        self.w = None
        self.r = {}
        self.sem = None
        self.dc = 0
        self.name = name


class T:
    def __init__(self, t, name):
        self.t = t
        self.b = Buf(name)

    def __getitem__(self, k):
        return self.t[k]


class KB:
    def __init__(self, nc, es):
        self.nc = nc
        self.es = es
        self.E = {"pe": nc.tensor, "act": nc.scalar, "dve": nc.vector, "pool": nc.gpsimd, "sp": nc.sync}
        self.esem = {}
        self.ecnt = {}
        for e in ("pe", "act", "dve", "pool"):
            self.esem[e] = es.enter_context(nc.semaphore("sem_" + e))
            self.ecnt[e] = 0
        self.seen = {e: {} for e in self.E}
        self.dbufs = []
        self.n = 0

    def sb(self, es, name, shape, dt=F32):
        return T(es.enter_context(self.nc.sbuf_tensor("s_" + name, shape, dt)), name)

    def ps(self, es, name, shape, dt=F32):
        return T(es.enter_context(self.nc.psum_tensor("p_" + name, shape, dt)), name)

    def _wait(self, e, rec):
        sem, val = rec
        k = id(sem)
        if self.seen[e].get(k, 0) >= val:
            return
        self.E[e].wait_ge(sem, val)
        self.seen[e][k] = val

    def _dep(self, e, rec):
        if e == "pe" and rec[0] is self.esem["pe"]:
            return
        self._wait(e, rec)

    def _deps(self, e, reads, writes):
        own = self.esem.get(e)
        for b in reads:
            if b.w is not None:
                self._dep(e, b.w)
        for b in writes:
            if b.w is not None and b.w[0] is not own:
                self._dep(e, b.w)
            for rec in b.r.values():
                if rec[0] is not own:
                    self._dep(e, rec)

    def op(self, e, fn, reads=(), writes=()):
        reads = [x.b if isinstance(x, T) else x for x in reads]
        writes = [x.b if isinstance(x, T) else x for x in writes]
        self._deps(e, reads, writes)
        ins = fn(self.E[e])
        self.ecnt[e] += 1
        ins.then_inc(self.esem[e], 1)
        rec = (self.esem[e], self.ecnt[e])
        for b in reads:
            b.r[id(rec[0])] = rec
        for b in writes:
            b.w = rec
            b.r = {}
        self.n += 1
        return ins

    def dma(self, out, in_, reads=(), writes=(), sbuf=None, q=None):
        if q is None:
            q = "pool" if str(out.space) == "DRAM" else "sp"
        reads = [x.b if isinstance(x, T) else x for x in reads]
        writes = [x.b if isinstance(x, T) else x for x in writes]
        b = sbuf.b if isinstance(sbuf, T) else sbuf
        if b.sem is None:
            b.sem = self.es.enter_context(self.nc.semaphore("dsem%d" % len(self.dbufs)))
            self.dbufs.append(b)
        self._deps(q, reads, writes)
        if b.dc > 0:
            self._wait(q, (b.sem, b.dc))
        ins = self.E[q].dma_start(out=out, in_=in_)
        b.dc += 16
        ins.then_inc(b.sem, 16)
        rec = (b.sem, b.dc)
        for x in reads:
            x.r[id(rec[0])] = rec
        for x in writes:
            x.w = rec
            x.r = {}
        self.n += 1
        return ins

    def barrier(self, engines=("pe", "act", "dve", "pool", "sp")):
        for e in engines:
            for s in ("pe", "act", "dve", "pool"):
                if self.ecnt[s] > 0 and not (e == s):
                    self._wait(e, (self.esem[s], self.ecnt[s]))
            for b in self.dbufs:
                if b.dc > 0:
                    self._wait(e, (b.sem, b.dc))


def build(S, dbg=False, stop_after=9):
    NT = S // 128
    NB = S // 256
    NST = S // 256
    nc = bass.Bass("TRN2", target_bir_lowering=False)
    es = contextlib.ExitStack()
    kb = KB(nc, es)

    def din(name, shape, dt=F32):
        return nc.dram_tensor(name, shape, dt, kind="ExternalInput").ap()

    def dscr(name, shape, dt):
        if dbg:
            return nc.dram_tensor(name, shape, dt, kind="ExternalOutput").ap()
        return nc.dram_tensor(name, shape, dt).ap()

    x_d = din("x", [S, D])
    mem_d = din("mem", [256, D])
    w_in_d = din("w_in", [D, 4608])
    gproj_d = din("gproj", [512, D])
    mproj_d = din("mproj", [512, D])
    wout_d = din("w_out", [D, D])
    xwq_d = din("xa_wq", [D, 512])
    xwkv_d = din("xa_wkv", [D, D])
    xwo_d = din("xa_wo", [512, D])
    pwq_d = din("peer_wq", [D, 2048])
    gains_d = din("gains", [128, 4, 8])
    gng_d = din("gng", [128, 512])
    bs_d = din("bs", [128, 8])
    fg_d = din("fg", [128, D])
    wmT_d = din("wmT", [128, 8, 128])
    skT_d = din("skT", [128, 16, 128])
    wdT_d = din("wdT", [128, 131072])
    wu_d = din("wu", [128, 131072])
    ident_d = din("ident", [128, 128])
    tri_d = din("tri", [128, 128])
    iota_d = din("iota", [128, 128])
    cos_d = din("cos", [128, NT, 32])
    sin_d = din("sin", [128, NT, 32])
    ind_d = din("ind", [32, S], BF16)
    pastneg_d = din("pastneg", [128, NT, 32])
    ownind_d = din("ownind", [128, NT, 32])
    out_d = nc.dram_tensor("out", [S, D], F32, kind="ExternalOutput").ap()

    QT_d = dscr("QT", [512, S], BF16)
    KT_d = dscr("KT", [512, S], BF16)
    VS_d = dscr("VS", [NT, 128, 8, 65], BF16)
    SIGA_d = dscr("SIGA", [S, D], BF16)
    MB_d = dscr("MB", [S, D], F32)
    ATT_d = dscr("ATT", [S, 512], BF16)
    X2_d = dscr("X2", [S, D], F32)
    WD_d = dscr("WD", [64, 128, 2048], BF16)
    WU_d = dscr("WU", [64, 128, 2048], BF16)
    dQT, dKT, dVS, dSIGA, dMB, dATT, dX2 = (Buf(n) for n in ("QT", "KT", "VS", "SIGA", "MB", "ATT", "X2"))
    dWD = [Buf("WD%d" % i) for i in range(64)]
    dWU = [Buf("WU%d" % i) for i in range(64)]
    DR = Buf("dram_const")

    identf = kb.sb(es, "identf", [128, 128])
    identb = kb.sb(es, "identb", [128, 128], BF16)
    trib = kb.sb(es, "trib", [128, 128], BF16)
    trif = kb.sb(es, "trif", [128, 128])
    gains = kb.sb(es, "gains", [128, 4, 8])
    qsq = kb.sb(es, "qsq", [128, NT, 8])
    kmax2 = kb.sb(es, "kmax2", [128, 8])
    negshift = kb.sb(es, "negshift", [128, NT, 8])
    stg = [None, None]
    cvo = [None, None]

    def alloc_stg(pes, with_cvo=False):
        for i in range(2):
            stg[i] = kb.sb(pes, "stg%d_%d" % (i, kb.n), [128, 2048])
            if with_cvo:
                cvo[i] = kb.sb(pes, "cvo%d_%d" % (i, kb.n), [128, 2048], BF16)

    kb.dma(identf[:], ident_d[:, :], writes=[identf], sbuf=identf)
    kb.dma(trif[:], tri_d[:, :], writes=[trif], sbuf=trif)
    kb.dma(gains[:], gains_d[:, :, :], writes=[gains], sbuf=gains)
    kb.op("dve", lambda e: e.tensor_copy(out=identb[:], in_=identf[:]), [identf], [identb])
    kb.op("dve", lambda e: e.tensor_copy(out=trib[:], in_=trif[:]), [trif], [trib])
    kb.op("dve", lambda e: e.memset(kmax2[:], 0.0), [], [kmax2])

    cast_ctr = [0]

    def cast(out_ap, in_ap, reads, writes, eng=None):
        if eng is None:
            eng = ("act", "dve")[cast_ctr[0] % 2]
            cast_ctr[0] += 1
        if eng == "act":
            kb.op("act", lambda e: e.activation(out=out_ap, in_=in_ap, func=AF.Copy), reads, writes)
        else:
            kb.op(eng, lambda e: e.tensor_copy(out=out_ap, in_=in_ap), reads, writes)

    def load_w(dst, src_d, K, N, scale=None):
        for kc in range(K // 128):
            for c0 in range(0, N, 2048):
                w = min(2048, N - c0)
                s = stg[cast_ctr[0] % 2]
                kb.dma(s[:, 0:w], src_d[kc * 128:(kc + 1) * 128, c0:c0 + w], writes=[s], sbuf=s)
                if scale is None:
                    cast(dst[:, kc, c0:c0 + w], s[:, 0:w], [s], [dst])
                else:
                    cast_ctr[0] += 1
                    kb.op("dve", lambda e: e.tensor_single_scalar(out=dst[:, kc, c0:c0 + w], in_=s[:, 0:w], scalar=scale, op=ALU.mult),
                          [s], [dst])

    def conv_gen():
        for which, (src, dst, dbl) in enumerate(((wdT_d, WD_d, dWD), (wu_d, WU_d, dWU))):
            for c in range(64):
                k = c % 2
                s = stg[k]
                o = cvo[k]
                kb.dma(s[:], src[:, c * 2048:(c + 1) * 2048], writes=[s], sbuf=s)
                yield
                kb.op("dve", lambda e: e.tensor_copy(out=o[:], in_=s[:]), [s], [o])
                kb.dma(dst[c], o[:], reads=[o], writes=[dbl[c]], sbuf=o)
                yield

    def phase1():
        with contextlib.ExitStack() as pes:
            alloc_stg(pes)
            w_in = kb.sb(pes, "w_in_sb", [128, KC, 4608], BF16)
            gproj = kb.sb(pes, "gproj_sb", [128, 4, D], BF16)
            wmT = kb.sb(pes, "wmT_sb", [128, 8, 128], BF16)
            wmTf = kb.sb(pes, "wmTf", [128, 8, 128])
            cos = kb.sb(pes, "cos_sb", [128, NT, 32])
            sin = kb.sb(pes, "sin_sb", [128, NT, 32])
            gng = kb.sb(pes, "gng_sb", [128, 512])
            bs = kb.sb(pes, "bs_sb", [128, 8])
            kb.dma(cos[:], cos_d[:, :, :], writes=[cos], sbuf=cos)
            kb.dma(sin[:], sin_d[:, :, :], writes=[sin], sbuf=sin)
            kb.dma(gng[:], gng_d[:, :], writes=[gng], sbuf=gng)
            kb.dma(bs[:], bs_d[:, :], writes=[bs], sbuf=bs)
            kb.dma(wmTf[:], wmT_d[:, :, :], writes=[wmTf], sbuf=wmTf)
            kb.op("dve", lambda e: e.tensor_tensor(out=wmT[:], in0=wmTf[:],
                                                   in1=trif[:].unsqueeze(1).to_broadcast([128, 8, 128]),
                                                   op=ALU.mult), [wmTf, trif], [wmT])
            load_w(w_in, w_in_d, D, 4608)
            load_w(gproj, gproj_d, 512, D, scale=0.5)

            xs = [kb.sb(pes, "xs%d" % i, [128, D]) for i in range(3)]
            junk = kb.sb(pes, "junk", [128, D], BF16)
            ssq = [kb.sb(pes, "ssq%d" % i, [128, 1]) for i in range(2)]
            rstd = [kb.sb(pes, "rstd%d" % i, [128, 1]) for i in range(2)]
            xh = [kb.sb(pes, "xh%d" % i, [128, D], BF16) for i in range(2)]
            xT = [kb.sb(pes, "xT%d" % i, [128, KC, 128], BF16) for i in range(2)]
            t1 = kb.sb(pes, "t1", [128, 8, 32])
            t2 = kb.sb(pes, "t2", [128, 8, 32])
            sqt = kb.sb(pes, "sqt", [128, 8, 64])
            ksq = kb.sb(pes, "ksq", [128, 8])
            qr = [kb.sb(pes, "qr%d" % i, [128, 8, 64], BF16) for i in range(2)]
            kr = [kb.sb(pes, "kr%d" % i, [128, 8, 64], BF16) for i in range(2)]
            va = [kb.sb(pes, "va%d" % i, [128, 8, 65], BF16) for i in range(2)]
            u = [kb.sb(pes, "u%d" % i, [128, 512]) for i in range(2)]
            gv = kb.sb(pes, "gv", [128, 512])
            ssv = kb.sb(pes, "ssv", [128, 1])
            rsv = kb.sb(pes, "rsv", [128, 1])
            vh = [kb.sb(pes, "vh%d" % i, [128, 512], BF16) for i in range(2)]
            siga = [kb.sb(pes, "siga%d" % i, [128, D], BF16) for i in range(2)]
            sigb = [kb.sb(pes, "sigb%d" % i, [128, D]) for i in range(2)]
            qkT = [kb.sb(pes, "qkT%d" % i, [128, 8, 128], BF16) for i in range(2)]
            z1 = kb.sb(pes, "z1", [128, 512])
            sg = [kb.sb(pes, "sg%d" % i, [128, 512], BF16) for i in range(2)]
            sgT = [kb.sb(pes, "sgT%d" % i, [128, 4, 128], BF16) for i in range(2)]
            mb = [kb.sb(pes, "mb%d" % i, [128, D]) for i in range(2)]
            for i in range(2):
                kb.op("dve", lambda e: e.memset(va[i][:], 1.0), [], [va[i]])

            pT = kb.ps(pes, "pT", [128, 1024], BF16)
            pQK = kb.ps(pes, "pQK", [128, 1024], BF16)
            pZ = kb.ps(pes, "pZ", [128, 512])
            pST = kb.ps(pes, "pST", [128, 1024], BF16)
            pP = [kb.ps(pes, "pP%d" % i, [128, 512]) for i in range(4)]
            pctr = [0]

            def next_p():
                p = pP[pctr[0] % 4]
                pctr[0] += 1
                return p

            def pre_load(i):
                kb.dma(xs[i % 3][:], x_d[i * 128:(i + 1) * 128, :], reads=[DR], writes=[xs[i % 3]], sbuf=xs[i % 3])

            def pre_sq(i):
                s = i % 2
                kb.op("act", lambda e: e.activation(out=junk[:], in_=xs[i % 3][:], func=AF.Square, accum_out=ssq[s][:]),
                      [xs[i % 3]], [junk, ssq[s]])

            def pre_sqrt(i):
                s = i % 2
                kb.op("act", lambda e: e.activation(out=rstd[s][:], in_=ssq[s][:], func=AF.Sqrt, bias=EPS, scale=1.0 / D),
                      [ssq[s]], [rstd[s]])
                kb.op("dve", lambda e: e.reciprocal(out=rstd[s][:], in_=rstd[s][:]), [rstd[s]], [rstd[s]])

            def pre_xh(i):
                s = i % 2
                kb.op("act", lambda e: e.activation(out=xh[s][:], in_=xs[i % 3][:], func=AF.Copy, scale=rstd[s][:]),
                      [xs[i % 3], rstd[s]], [xh[s]])

            def pre_b(i):
                s = i % 2
                for kc in range(KC):
                    kb.op("pe", lambda e: e.transpose(out=pT[:, kc * 128:(kc + 1) * 128], in_=xh[s][:, kc * 128:(kc + 1) * 128],
                                                      identity=identb[:]), [xh[s], identb], [pT])
                kb.op("dve", lambda e: e.tensor_tensor(out=xT[s][:], in0=pT[:].rearrange("p (k t) -> p k t", k=KC),
                                                       in1=gains[:, 0, :].unsqueeze(2).to_broadcast([128, KC, 128]),
                                                       op=ALU.mult), [pT, gains], [xT[s]])

            def mm_group(i, n):
                s = i % 2
                p = next_p()
                for kc in range(KC):
                    kb.op("pe", lambda e: e.matmul(p[:], lhsT=xT[s][:, kc, :], rhs=w_in[:, kc, n * 512:(n + 1) * 512],
                                                   start=(kc == 0), stop=(kc == KC - 1)), [xT[s], w_in], [p])
                return p

            def rope(p, dst, i):
                pv = p[:].rearrange("p (h d) -> p h d", h=8)
                x1 = pv[:, :, 0:32]
                x2 = pv[:, :, 32:64]
                cb = cos[:, i, :].unsqueeze(1).to_broadcast([128, 8, 32])
                sbb = sin[:, i, :].unsqueeze(1).to_broadcast([128, 8, 32])
                kb.op("dve", lambda e: e.tensor_tensor(out=t1[:], in0=x1, in1=cb, op=ALU.mult), [p, cos], [t1])
                kb.op("dve", lambda e: e.tensor_tensor(out=t2[:], in0=x2, in1=sbb, op=ALU.mult), [p, sin], [t2])
                kb.op("dve", lambda e: e.tensor_tensor(out=dst[:, :, 0:32], in0=t1[:], in1=t2[:], op=ALU.subtract), [t1, t2], [dst])
                kb.op("dve", lambda e: e.tensor_tensor(out=t1[:], in0=x2, in1=cb, op=ALU.mult), [p, cos], [t1])
                kb.op("dve", lambda e: e.tensor_tensor(out=t2[:], in0=x1, in1=sbb, op=ALU.mult), [p, sin], [t2])
                kb.op("dve", lambda e: e.tensor_tensor(out=dst[:, :, 32:64], in0=t1[:], in1=t2[:], op=ALU.add), [t1, t2], [dst])

            def tail2(i):
                s = i % 2
                for j in range(4):
                    kb.op("pe", lambda e: e.transpose(out=pST[:, j * 128:(j + 1) * 128], in_=sg[s][:, j * 128:(j + 1) * 128],
                                                      identity=identb[:]), [sg[s], identb], [pST])
                kb.op("act", lambda e: e.activation(out=sgT[s][:], in_=pST[:, 0:512].rearrange("p (k t) -> p k t", k=4), func=AF.Copy),
                      [pST], [sgT[s]])
                for half in range(2):
                    p = next_p()
                    for kc in range(4):
                        kb.op("pe", lambda e: e.matmul(p[:], lhsT=sgT[s][:, kc, :], rhs=gproj[:, kc, half * 512:(half + 1) * 512],
                                                       start=(kc == 0), stop=(kc == 3)), [sgT[s], gproj], [p])
                    kb.op("dve", lambda e: e.scalar_tensor_tensor(out=mb[s][:, half * 512:(half + 1) * 512],
                                                                  in0=sigb[s][:, half * 512:(half + 1) * 512], scalar=1.0, in1=p[:],
                                                                  op0=ALU.add, op1=ALU.mult), [p, sigb[s]], [mb[s]])
                kb.dma(MB_d[i * 128:(i + 1) * 128, :], mb[s][:], reads=[mb[s]], writes=[dMB], sbuf=mb[s])

            pre_load(0)
            if NT > 1:
                pre_load(1)
            pre_sq(0)
            pre_sqrt(0)
            pre_xh(0)
            pre_b(0)
            for i in range(NT):
                s = i % 2
                if i + 2 < NT:
                    pre_load(i + 2)
                p = mm_group(i, 0)
                kb.op("act", lambda e: e.activation(out=sqt[:], in_=p[:].rearrange("p (h d) -> p h d", h=8), func=AF.Square), [p], [sqt])
                kb.op("dve", lambda e: e.tensor_reduce(out=qsq[:, i, :], in_=sqt[:], axis=AX.X, op=ALU.add), [sqt], [qsq])
                rope(p, qr[s], i)
                p = mm_group(i, 1)
                kb.op("act", lambda e: e.activation(out=sqt[:], in_=p[:].rearrange("p (h d) -> p h d", h=8), func=AF.Square), [p], [sqt])
                kb.op("dve", lambda e: e.tensor_reduce(out=ksq[:], in_=sqt[:], axis=AX.X, op=ALU.add), [sqt], [ksq])
                kb.op("dve", lambda e: e.tensor_tensor(out=kmax2[:], in0=kmax2[:], in1=ksq[:], op=ALU.max), [kmax2, ksq], [kmax2])
                rope(p, kr[s], i)
                p = mm_group(i, 2)
                kb.op("act", lambda e: e.activation(out=va[s][:, :, 0:64], in_=p[:].rearrange("p (h d) -> p h d", h=8), func=AF.Copy),
                      [p], [va[s]])
                kb.dma(VS_d[i], va[s][:], reads=[va[s]], writes=[dVS], sbuf=va[s])
                p = mm_group(i, 3)
                kb.op("act", lambda e: e.activation(out=u[s][:], in_=p[:], func=GELU), [p], [u[s]])
                if i > 0:
                    tail2(i - 1)
                p = mm_group(i, 4)
                kb.op("act", lambda e: e.activation(out=gv[:], in_=p[:], func=GELU), [p], [gv])
                kb.op("act", lambda e: e.activation(out=junk[:, 0:512], in_=gv[:], func=AF.Square, accum_out=ssv[:]), [gv], [junk, ssv])
                if i + 1 < NT:
                    pre_sq(i + 1)
                kb.op("act", lambda e: e.activation(out=rsv[:], in_=ssv[:], func=AF.Sqrt, bias=EPS, scale=1.0 / 512), [ssv], [rsv])
                if i + 1 < NT:
                    pre_sqrt(i + 1)
                kb.op("dve", lambda e: e.reciprocal(out=rsv[:], in_=rsv[:]), [rsv], [rsv])
                kb.op("act", lambda e: e.activation(out=vh[s][:], in_=gv[:], func=AF.Copy, scale=rsv[:]), [gv, rsv], [vh[s]])
                if i + 1 < NT:
                    pre_xh(i + 1)
                for n in (5, 6):
                    p = mm_group(i, n)
                    kb.op("act", lambda e: e.activation(out=siga[s][:, (n - 5) * 512:(n - 4) * 512], in_=p[:], func=AF.Tanh, scale=0.5),
                          [p], [siga[s]])
                kb.dma(SIGA_d[i * 128:(i + 1) * 128, :], siga[s][:], reads=[siga[s]], writes=[dSIGA], sbuf=siga[s])
                if i + 1 < NT:
                    pre_b(i + 1)
                for n in (7, 8):
                    p = mm_group(i, n)
                    kb.op("act", lambda e: e.activation(out=sigb[s][:, (n - 7) * 512:(n - 6) * 512], in_=p[:], func=AF.Tanh, scale=0.5),
                          [p], [sigb[s]])
                for j in range(4):
                    kb.op("pe", lambda e: e.transpose(out=pQK[:, j * 128:(j + 1) * 128],
                                                      in_=qr[s][:].rearrange("p h d -> p (h d)")[:, j * 128:(j + 1) * 128],
                                                      identity=identb[:]), [qr[s], identb], [pQK])
                for j in range(4):
                    kb.op("pe", lambda e: e.transpose(out=pQK[:, 512 + j * 128:512 + (j + 1) * 128],
                                                      in_=kr[s][:].rearrange("p h d -> p (h d)")[:, j * 128:(j + 1) * 128],
                                                      identity=identb[:]), [kr[s], identb], [pQK])
                kb.op("dve", lambda e: e.tensor_copy(out=qkT[s][:], in_=pQK[:].rearrange("p (k t) -> p k t", k=8)), [pQK], [qkT[s]])
                kb.dma(QT_d.rearrange("(j p) s -> p j s", p=128)[:, :, i * 128:(i + 1) * 128], qkT[s][:, 0:4, :],
                       reads=[qkT[s]], writes=[dQT], sbuf=qkT[s])
                kb.dma(KT_d.rearrange("(j p) s -> p j s", p=128)[:, :, i * 128:(i + 1) * 128], qkT[s][:, 4:8, :],
                       reads=[qkT[s]], writes=[dKT], sbuf=qkT[s])
                for g in range(8):
                    kb.op("pe", lambda e: e.matmul(pZ[:, g * 64:(g + 1) * 64], lhsT=wmT[:, g, :], rhs=vh[s][:, g * 64:(g + 1) * 64],
                                                   start=True, stop=True), [wmT, vh[s]], [pZ])
                kb.op("dve", lambda e: e.tensor_tensor(out=z1[:], in0=pZ[:], in1=gng[:], op=ALU.mult), [pZ, gng], [z1])
                kb.op("dve", lambda e: e.tensor_tensor(out=z1[:].rearrange("p (g d) -> p g d", g=8),
                                                       in0=z1[:].rearrange("p (g d) -> p g d", g=8),
                                                       in1=bs[:].unsqueeze(2).to_broadcast([128, 8, 64]), op=ALU.add), [z1, bs], [z1])
                kb.op("dve", lambda e: e.tensor_tensor(out=sg[s][:], in0=z1[:], in1=u[s][:], op=ALU.mult), [z1, u[s]], [sg[s]])
            tail2(NT - 1)

            pk = pP[0]
            kT8 = kb.sb(pes, "kT8", [8, 128])
            km8 = kb.sb(pes, "km8", [8, 1])
            dg8 = kb.sb(pes, "dg8", [8, 8])
            ones8 = kb.sb(pes, "ones8", [8, 128])
            KM = kb.sb(pes, "KM", [128, 8])
            kb.op("pe", lambda e: e.transpose(out=pk[0:8, 0:128], in_=kmax2[:], identity=identf[:]), [kmax2, identf], [pk])
            kb.op("dve", lambda e: e.tensor_copy(out=kT8[:], in_=pk[0:8, 0:128]), [pk], [kT8])
            kb.op("dve", lambda e: e.tensor_reduce(out=km8[:], in_=kT8[:], axis=AX.X, op=ALU.max), [kT8], [km8])
            kb.op("dve", lambda e: e.tensor_single_scalar(out=dg8[:], in_=identf[0:8, 0:8], scalar=km8[:, 0:1], op=ALU.mult),
                  [identf, km8], [dg8])
            kb.op("dve", lambda e: e.memset(ones8[:], 1.0), [], [ones8])
            kb.op("pe", lambda e: e.matmul(pk[:, 256:264], lhsT=ones8[:], rhs=dg8[:], start=True, stop=True), [ones8, dg8], [pk])
            kb.op("dve", lambda e: e.tensor_copy(out=KM[:], in_=pk[:, 256:264]), [pk], [KM])
            kb.op("dve", lambda e: e.tensor_tensor(out=negshift[:], in0=qsq[:], in1=KM[:].unsqueeze(1).to_broadcast([128, NT, 8]),
                                                   op=ALU.mult), [qsq, KM], [negshift])
            kb.op("act", lambda e: e.activation(out=negshift[:], in_=negshift[:], func=AF.Sqrt, scale=1.0404), [negshift], [negshift])
            kb.op("dve", lambda e: e.tensor_single_scalar(out=negshift[:], in_=negshift[:], scalar=-1.0, op=ALU.mult),
                  [negshift], [negshift])
            kb.barrier()

    def phase2():
        conv = conv_gen()

        def conv_step(n=1):
            for _ in range(n):
                try:
                    next(conv)
                except StopIteration:
                    return

        with contextlib.ExitStack() as pes:
            alloc_stg(pes, True)
            KA = [kb.sb(pes, "KA%d" % i, [96, S], BF16) for i in range(2)]
            QA = [kb.sb(pes, "QA%d" % i, [96, S], BF16) for i in range(2)]
            VA = [kb.sb(pes, "VA%d" % i, [128, NT, 65], BF16) for i in range(2)]
            AO = [kb.sb(pes, "AO%d" % i, [128, NT, 64], BF16) for i in range(2)]
            pastneg = kb.sb(pes, "pastneg", [128, NT, 32])
            ownind = kb.sb(pes, "ownind", [128, NT, 32])
            km32 = kb.sb(pes, "km32", [64, NB])
            kmT = kb.sb(pes, "kmT", [64, 32], BF16)
            gm = kb.sb(pes, "gm", [128, 16, 32])
            m8 = kb.sb(pes, "m8", [128, 16, 8])
            sel = kb.sb(pes, "sel", [128, 16, 32])
            bias2 = kb.sb(pes, "bias2", [128, 16, 96])
            PT = [kb.sb(pes, "PT%d" % i, [128, 512], BF16) for i in range(4)]
            rz = kb.sb(pes, "rz", [128, 1])
            pSb = [kb.ps(pes, "pS%d" % i, [128, 512]) for i in range(4)]
            pG = kb.ps(pes, "pG", [128, 512])
            pO = [kb.ps(pes, "pO%d" % i, [128, 512]) for i in range(2)]
            kb.dma(pastneg[:], pastneg_d[:, :, :], writes=[pastneg], sbuf=pastneg)
            kb.dma(ownind[:], ownind_d[:, :, :], writes=[ownind], sbuf=ownind)
            kb.op("dve", lambda e: e.memset(bias2[:], 0.0), [], [bias2])
            kb.op("dve", lambda e: e.memset(kmT[:], 0.0), [], [kmT])
            for i in range(2):
                kb.dma(KA[i][64:96, :], ind_d[:, :], writes=[KA[i]], sbuf=KA[i])
            scale = 0.125
            CB = 16
            sctr = [0]
            def head_loads(h):
                s = h % 2
                kb.dma(KA[s][0:64, :], KT_d[h * 64:(h + 1) * 64, :], reads=[dKT], writes=[KA[s]], sbuf=KA[s])
                kb.dma(QA[s][0:64, :], QT_d[h * 64:(h + 1) * 64, :], reads=[dQT], writes=[QA[s]], sbuf=QA[s])
                kb.dma(VA[s][:], VS_d.rearrange("t p h c -> p t h c")[:, :, h, :], reads=[dVS], writes=[VA[s]], sbuf=VA[s])

            head_loads(0)
            for h in range(8):
                s = h % 2
                kb.op("dve", lambda e: e.tensor_reduce(out=km32[:], in_=KA[s][0:64, :].rearrange("p (n l) -> p n l", l=256),
                                                       axis=AX.X, op=ALU.add), [KA[s]], [km32])
                kb.op("act", lambda e: e.activation(out=kmT[:, 0:NB], in_=km32[:], func=AF.Copy, scale=1.0 / 256), [km32], [kmT])
                for c0 in range(0, NT, CB):
                    nb_ = min(CB, NT - c0)
                    for cc in range(nb_):
                        c = c0 + cc
                        kb.op("pe", lambda e: e.matmul(pG[:, cc * 32:(cc + 1) * 32], lhsT=QA[s][0:64, c * 128:(c + 1) * 128],
                                                       rhs=kmT[:, :], start=True, stop=True), [QA[s], kmT], [pG])
                    kb.op("dve", lambda e: e.tensor_tensor(out=gm[:, 0:nb_, :], in0=pG[:, 0:nb_ * 32].rearrange("p (c n) -> p c n", n=32),
                                                           in1=pastneg[:, c0:c0 + nb_, :], op=ALU.add), [pG, pastneg], [gm])
                    for cc in range(nb_):
                        kb.op("dve", lambda e: e.max(out=m8[:, cc, :], in_=gm[:, cc, :]), [gm], [m8])
                    kb.op("dve", lambda e: e.tensor_tensor(out=sel[:, 0:nb_, :], in0=gm[:, 0:nb_, :],
                                                           in1=m8[:, 0:nb_, 2:3].to_broadcast([128, nb_, 32]), op=ALU.is_ge),
                          [gm, m8], [sel])
                    kb.op("dve", lambda e: e.tensor_tensor(out=sel[:, 0:nb_, :], in0=sel[:, 0:nb_, :],
                                                           in1=ownind[:, c0:c0 + nb_, :], op=ALU.max), [sel, ownind], [sel])
                    kb.op("dve", lambda e: e.tensor_scalar(out=sel[:, 0:nb_, :], in0=sel[:, 0:nb_, :], scalar1=-NEGM, scalar2=NEGM,
                                                           op0=ALU.mult, op1=ALU.add), [sel], [sel])
                    kb.op("dve", lambda e: e.tensor_tensor(out=bias2[:, 0:nb_, 64:96], in0=sel[:, 0:nb_, :],
                                                           in1=negshift[:, c0:c0 + nb_, h:h + 1].to_broadcast([128, nb_, 32]),
                                                           op=ALU.add), [sel, negshift], [bias2])
                    for c4 in range(0, nb_, 4):
                        pb = pSb[(c4 // 4) % 4]
                        n4 = min(4, nb_ - c4)
                        for cc in range(c4, c4 + n4):
                            kb.op("pe", lambda e: e.transpose(out=pb[0:96, (cc - c4) * 128:(cc - c4 + 1) * 128], in_=bias2[:, cc, :],
                                                              identity=identf[:]), [bias2, identf], [pb])
                        kb.op("act", lambda e: e.activation(out=QA[s][64:96, (c0 + c4) * 128:(c0 + c4 + n4) * 128],
                                                            in_=pb[64:96, 0:n4 * 128], func=AF.Copy), [pb], [QA[s]])
                if h + 1 < 8:
                    head_loads(h + 1)
                groups = []
                for c in range(NT):
                    js = list(range(c + 1))
                    for g0 in range(0, len(js), 4):
                        groups.append((c, js[g0:g0 + 4]))

                def emit_qk(gi):
                    c, js = groups[gi]
                    pb = pSb[sctr[0] % 4]
                    sctr[0] += 1
                    for jj, j in enumerate(js):
                        kb.op("pe", lambda e: e.matmul(pb[:, jj * 128:(jj + 1) * 128], lhsT=KA[s][0:96, j * 128:(j + 1) * 128],
                                                       rhs=QA[s][0:96, c * 128:(c + 1) * 128], start=True, stop=True),
                              [KA[s], QA[s]], [pb])
                    return pb

                LA = 3
                pend = [emit_qk(g_) for g_ in range(min(LA, len(groups)))]
                for gi in range(len(groups)):
                    c, js = groups[gi]
                    pb = pend.pop(0)
                    if gi + LA < len(groups):
                        pend.append(emit_qk(gi + LA))
                    pt = PT[gi % 4]
                    n = len(js)
                    kb.op("act", lambda e: e.activation(out=pt[:, 0:n * 128], in_=pb[:, 0:n * 128], func=AF.Exp, scale=scale),
                          [pb], [pt])
                    if js[-1] == c:
                        kb.op("dve", lambda e: e.tensor_tensor(out=pt[:, (n - 1) * 128:n * 128], in0=pt[:, (n - 1) * 128:n * 128],
                                                               in1=trib[:], op=ALU.mult), [pt, trib], [pt])
                    po = pO[c % 2]
                    for jj, j in enumerate(js):
                        kb.op("pe", lambda e: e.matmul(po[:, 0:65], lhsT=pt[:, jj * 128:(jj + 1) * 128], rhs=VA[s][:, j, :],
                                                       start=(j == 0), stop=(j == c)), [pt, VA[s]], [po])
                    if js[-1] == c:
                        kb.op("dve", lambda e: e.reciprocal(out=rz[:], in_=po[:, 64:65]), [po], [rz])
                        kb.op("dve", lambda e: e.tensor_single_scalar(out=AO[s][:, c, :], in_=po[:, 0:64], scalar=rz[:, 0:1],
                                                                      op=ALU.mult), [po, rz], [AO[s]])
                        if c % 4 == 3:
                            conv_step(2 if NT >= 64 else 40)
                kb.dma(ATT_d.rearrange("(t p) c -> p t c", p=128)[:, :, h * 64:(h + 1) * 64], AO[s][:], reads=[AO[s]],
                       writes=[dATT], sbuf=AO[s])
            conv_step(10000)
            kb.barrier()

    def phase3a():
        with contextlib.ExitStack() as pes:
            alloc_stg(pes)
            mproj = kb.sb(pes, "mproj_sb", [128, 4, D], BF16)
            wout = kb.sb(pes, "wout_sb", [128, KC, D], BF16)
            xwq = kb.sb(pes, "xwq_sb", [128, KC, 512], BF16)
            xwkv = kb.sb(pes, "xwkv_sb", [128, KC, D], BF16)
            xwo = kb.sb(pes, "xwo_sb", [128, 4, D], BF16)
            load_w(mproj, mproj_d, 512, D, scale=0.5)
            load_w(wout, wout_d, D, D)
            load_w(xwq, xwq_d, D, 512)
            load_w(xwkv, xwkv_d, D, D)
            load_w(xwo, xwo_d, 512, D)
            kmT = kb.sb(pes, "xkmT", [128, 4, 256], BF16)
            vm = kb.sb(pes, "xvm", [128, 2, 512], BF16)
            mT = kb.sb(pes, "mT", [128, KC, 256], BF16)
            xs = [kb.sb(pes, "x3s%d" % i, [128, D]) for i in range(3)]
            att = [kb.sb(pes, "att%d" % i, [128, 512], BF16) for i in range(3)]
            sga = [kb.sb(pes, "sga%d" % i, [128, D], BF16) for i in range(3)]
            mbs = [kb.sb(pes, "mbs%d" % i, [128, D]) for i in range(3)]
            two = lambda name, shape, dt=F32: [kb.sb(pes, "%s_%d" % (name, i), shape, dt) for i in range(2)]
            attT_ = two("attT", [128, 4, 128], BF16)
            tmpf_ = two("tmpf", [128, D])
            mrg_ = two("mrg", [128, D], BF16)
            mrgT_ = two("mrgT", [128, KC, 128], BF16)
            x1_ = two("x1", [128, D])
            junk_ = two("junk3", [128, D], BF16)
            ssq_ = two("ssq3", [128, 1])
            rstd_ = two("rstd3", [128, 1])
            xh_ = two("xh3", [128, D], BF16)
            xT_ = two("xT3", [128, KC, 128], BF16)
            qxT_ = two("qxT", [128, 4, 128], BF16)
            mx_ = two("mx", [128, 4])
            rs_ = two("rs", [128, 4])
            Pb_ = two("Pb", [128, 4, 256], BF16)
            PbT_ = two("PbT", [128, 8, 128], BF16)
            ob_ = two("ob", [128, 512], BF16)
            oT_ = two("oT", [128, 4, 128], BF16)
            x2 = [kb.sb(pes, "x2o%d" % i, [128, D]) for i in range(2)]
            pTb = [kb.ps(pes, "p3T%d" % i, [128, 1024], BF16) for i in range(2)]
            pF = [kb.ps(pes, "p3F%d" % i, [128, 512]) for i in range(6)]
            fctr = [0]
            tctr = [0]

            def nf():
                p = pF[fctr[0] % 6]
                fctr[0] += 1
                return p

            def nt():
                p = pTb[tctr[0] % 2]
                tctr[0] += 1
                return p

            def norm_T(src, sl):
                junk, ssq, rstd, xh = junk_[sl], ssq_[sl], rstd_[sl], xh_[sl]
                kb.op("act", lambda e: e.activation(out=junk[:], in_=src[:], func=AF.Square, accum_out=ssq[:]), [src], [junk, ssq])
                kb.op("act", lambda e: e.activation(out=rstd[:], in_=ssq[:], func=AF.Sqrt, bias=EPS, scale=1.0 / D), [ssq], [rstd])
                kb.op("dve", lambda e: e.reciprocal(out=rstd[:], in_=rstd[:]), [rstd], [rstd])
                kb.op("act", lambda e: e.activation(out=xh[:], in_=src[:], func=AF.Copy, scale=rstd[:]), [src, rstd], [xh])

            def norm_T2(sl):
                xh = xh_[sl]
                p = nt()
                for kc in range(KC):
                    kb.op("pe", lambda e: e.transpose(out=p[:, kc * 128:(kc + 1) * 128], in_=xh[:, kc * 128:(kc + 1) * 128],
                                                      identity=identb[:]), [xh, identb], [p])
                return p

            for mt in range(2):
                kb.dma(xs[mt][:], mem_d[mt * 128:(mt + 1) * 128, :], reads=[DR], writes=[xs[mt]], sbuf=xs[mt])
                norm_T(xs[mt], mt)
                p = norm_T2(mt)
                kb.op("dve", lambda e: e.tensor_tensor(out=mT[:, :, mt * 128:(mt + 1) * 128], in0=p[:].rearrange("p (k t) -> p k t", k=KC),
                                                       in1=gains[:, 2, :].unsqueeze(2).to_broadcast([128, KC, 128]), op=ALU.mult),
                      [p, gains], [mT])
            for hh in range(4):
                p = nf()
                for kc in range(KC):
                    kb.op("pe", lambda e: e.matmul(p[:, 0:256], lhsT=xwkv[:, kc, hh * 128:(hh + 1) * 128], rhs=mT[:, kc, :],
                                                   start=(kc == 0), stop=(kc == KC - 1)), [xwkv, mT], [p])
                kb.op("act", lambda e: e.activation(out=kmT[:, hh, :], in_=p[:, 0:256], func=AF.Copy), [p], [kmT])
            for mt in range(2):
                p = nf()
                for kc in range(KC):
                    kb.op("pe", lambda e: e.matmul(p[:], lhsT=mT[:, kc, mt * 128:(mt + 1) * 128], rhs=xwkv[:, kc, 512:1024],
                                                   start=(kc == 0), stop=(kc == KC - 1)), [xwkv, mT], [p])
                kb.op("act", lambda e: e.activation(out=vm[:, mt, :], in_=p[:], func=AF.Copy), [p], [vm])

            def loads(i):
                s = i % 3
                r = slice(i * 128, (i + 1) * 128)
                kb.dma(att[s][:], ATT_d[r, :], reads=[dATT], writes=[att[s]], sbuf=att[s])
                kb.dma(sga[s][:], SIGA_d[r, :], reads=[dSIGA], writes=[sga[s]], sbuf=sga[s])
                kb.dma(mbs[s][:], MB_d[r, :], reads=[dMB], writes=[mbs[s]], sbuf=mbs[s])
                kb.dma(xs[s][:], x_d[r, :], reads=[DR], writes=[xs[s]], sbuf=xs[s])

            def tile_gen(i):
                s = i % 2
                l3 = i % 3
                attT, tmpf, mrg, mrgT, x1 = attT_[s], tmpf_[s], mrg_[s], mrgT_[s], x1_[s]
                xT, qxT, mx, rs, Pb, PbT, ob, oT = xT_[s], qxT_[s], mx_[s], rs_[s], Pb_[s], PbT_[s], ob_[s], oT_[s]
                if i + 1 < NT:
                    loads(i + 1)
                yield
                p = nt()
                for j in range(4):
                    kb.op("pe", lambda e: e.transpose(out=p[:, j * 128:(j + 1) * 128], in_=att[l3][:, j * 128:(j + 1) * 128],
                                                      identity=identb[:]), [att[l3], identb], [p])
                kb.op("act", lambda e: e.activation(out=attT[:], in_=p[:, 0:512].rearrange("p (k t) -> p k t", k=4), func=AF.Copy),
                      [p], [attT])
                yield
                for half in range(2):
                    hs = slice(half * 512, (half + 1) * 512)
                    p = nf()
                    for kc in range(4):
                        kb.op("pe", lambda e: e.matmul(p[:], lhsT=attT[:, kc, :], rhs=mproj[:, kc, hs], start=(kc == 0), stop=(kc == 3)),
                              [attT, mproj], [p])
                    kb.op("dve", lambda e: e.scalar_tensor_tensor(out=tmpf[:, hs], in0=sga[l3][:, hs], scalar=1.0, in1=p[:],
                                                                  op0=ALU.add, op1=ALU.mult), [p, sga[l3]], [tmpf])
                    kb.op("dve", lambda e: e.tensor_tensor(out=mrg[:, hs], in0=tmpf[:, hs], in1=mbs[l3][:, hs], op=ALU.add),
                          [tmpf, mbs[l3]], [mrg])
                yield
                p = nt()
                for kc in range(KC):
                    kb.op("pe", lambda e: e.transpose(out=p[:, kc * 128:(kc + 1) * 128], in_=mrg[:, kc * 128:(kc + 1) * 128],
                                                      identity=identb[:]), [mrg, identb], [p])
                kb.op("act", lambda e: e.activation(out=mrgT[:], in_=p[:].rearrange("p (k t) -> p k t", k=KC), func=AF.Copy), [p], [mrgT])
                yield
                for half in range(2):
                    hs = slice(half * 512, (half + 1) * 512)
                    p = nf()
                    for kc in range(KC):
                        kb.op("pe", lambda e: e.matmul(p[:], lhsT=mrgT[:, kc, :], rhs=wout[:, kc, hs], start=(kc == 0), stop=(kc == KC - 1)),
                              [mrgT, wout], [p])
                    kb.op("dve", lambda e: e.tensor_tensor(out=x1[:, hs], in0=p[:], in1=xs[l3][:, hs], op=ALU.add), [p, xs[l3]], [x1])
                norm_T(x1, s)
                yield
                p = norm_T2(s)
                kb.op("dve", lambda e: e.tensor_tensor(out=xT[:], in0=p[:].rearrange("p (k t) -> p k t", k=KC),
                                                       in1=gains[:, 1, :].unsqueeze(2).to_broadcast([128, KC, 128]), op=ALU.mult),
                      [p, gains], [xT])
                yield
                p = nf()
                for hh in range(4):
                    for kc in range(KC):
                        kb.op("pe", lambda e: e.matmul(p[:, hh * 128:(hh + 1) * 128], lhsT=xwq[:, kc, hh * 128:(hh + 1) * 128],
                                                       rhs=xT[:, kc, :], start=(kc == 0), stop=(kc == KC - 1)), [xwq, xT], [p])
                kb.op("act", lambda e: e.activation(out=qxT[:], in_=p[:].rearrange("p (k t) -> p k t", k=4), func=AF.Copy,
                                                    scale=128.0 ** -0.5), [p], [qxT])
                yield
                psc = [nf(), nf()]
                for hh in range(4):
                    pp = psc[hh // 2]
                    kb.op("pe", lambda e: e.matmul(pp[:, (hh % 2) * 256:(hh % 2 + 1) * 256], lhsT=qxT[:, hh, :], rhs=kmT[:, hh, :],
                                                   start=True, stop=True), [qxT, kmT], [pp])
                for half in range(2):
                    kb.op("dve", lambda e: e.tensor_reduce(out=mx[:, half * 2:half * 2 + 2],
                                                           in_=psc[half][:].rearrange("p (h m) -> p h m", h=2), axis=AX.X, op=ALU.max),
                          [psc[half]], [mx])
                kb.op("dve", lambda e: e.tensor_single_scalar(out=mx[:], in_=mx[:], scalar=-1.0, op=ALU.mult), [mx], [mx])
                for hh in range(4):
                    pp = psc[hh // 2]
                    kb.op("act", lambda e: e.activation(out=Pb[:, hh, :], in_=pp[:, (hh % 2) * 256:(hh % 2 + 1) * 256], func=AF.Exp,
                                                        bias=mx[:, hh:hh + 1], accum_out=rs[:, hh:hh + 1]), [pp, mx], [Pb, rs])
                yield
                p = nt()
                for hh in range(4):
                    for mt in range(2):
                        kb.op("pe", lambda e: e.transpose(out=p[:, (hh * 2 + mt) * 128:(hh * 2 + mt + 1) * 128],
                                                          in_=Pb[:, hh, mt * 128:(mt + 1) * 128], identity=identb[:]), [Pb, identb], [p])
                kb.op("act", lambda e: e.activation(out=PbT[:], in_=p[:].rearrange("p (k t) -> p k t", k=8), func=AF.Copy), [p], [PbT])
                yield
                p = nf()
                for hh in range(4):
                    for mt in range(2):
                        kb.op("pe", lambda e: e.matmul(p[:, hh * 128:(hh + 1) * 128], lhsT=PbT[:, hh * 2 + mt, :],
                                                       rhs=vm[:, mt, hh * 128:(hh + 1) * 128], start=(mt == 0), stop=(mt == 1)),
                              [PbT, vm], [p])
                kb.op("dve", lambda e: e.reciprocal(out=rs[:], in_=rs[:]), [rs], [rs])
                kb.op("dve", lambda e: e.tensor_tensor(out=ob[:].rearrange("p (h d) -> p h d", h=4),
                                                       in0=p[:].rearrange("p (h d) -> p h d", h=4),
                                                       in1=rs[:].unsqueeze(2).to_broadcast([128, 4, 128]), op=ALU.mult), [p, rs], [ob])
                yield
                p = nt()
                for j in range(4):
                    kb.op("pe", lambda e: e.transpose(out=p[:, j * 128:(j + 1) * 128], in_=ob[:, j * 128:(j + 1) * 128],
                                                      identity=identb[:]), [ob, identb], [p])
                kb.op("act", lambda e: e.activation(out=oT[:], in_=p[:, 0:512].rearrange("p (k t) -> p k t", k=4), func=AF.Copy), [p], [oT])
                yield
                for half in range(2):
                    hs = slice(half * 512, (half + 1) * 512)
                    p = nf()
                    for kc in range(4):
                        kb.op("pe", lambda e: e.matmul(p[:], lhsT=oT[:, kc, :], rhs=xwo[:, kc, hs], start=(kc == 0), stop=(kc == 3)),
                              [oT, xwo], [p])
                    kb.op("dve", lambda e: e.tensor_tensor(out=x2[s][:, hs], in0=p[:], in1=x1[:, hs], op=ALU.add), [p, x1], [x2[s]])
                kb.dma(X2_d[i * 128:(i + 1) * 128, :], x2[s][:], reads=[x2[s]], writes=[dX2], sbuf=x2[s])

            loads(0)
            active = []
            nxt = 0
            rounds = 0
            while nxt < NT or active:
                if nxt < NT and len(active) < 2 and (not active or rounds % 13 == 6 or len(active) == 0):
                    active.append(tile_gen(nxt))
                    nxt += 1
                for g_ in list(active):
                    try:
                        next(g_)
                    except StopIteration:
                        active.remove(g_)
                rounds += 1
            kb.barrier()

    def phase3b():
        WPQ_d = nc.dram_tensor("WPQ", [128, KC, 2048], BF16).ap()
        dWPQ = Buf("WPQ")
        with contextlib.ExitStack() as pes:
            skT = kb.sb(pes, "skT_sb", [128, 16, 128], BF16)
            iot = kb.sb(pes, "iot", [128, 128])
            fg = kb.sb(pes, "fg_sb", [128, D])
            with contextlib.ExitStack() as ies:
                alloc_stg(ies, True)
                for kc in range(KC):
                    k = kc % 2
                    kb.dma(stg[k][:], pwq_d[kc * 128:(kc + 1) * 128, :], writes=[stg[k]], sbuf=stg[k])
                    cast(cvo[k][:], stg[k][:], [stg[k]], [cvo[k]])
                    kb.dma(WPQ_d[:, kc, :], cvo[k][:], reads=[cvo[k]], writes=[dWPQ], sbuf=cvo[k])
                kb.dma(stg[0][:], skT_d.rearrange("p a n -> p (a n)"), writes=[stg[0]], sbuf=stg[0])
                kb.op("dve", lambda e: e.tensor_copy(out=skT[:].rearrange("p a n -> p (a n)"), in_=stg[0][:]), [stg[0]], [skT])
                kb.barrier()
            kb.dma(iot[:], iota_d[:, :], writes=[iot], sbuf=iot)
            kb.dma(fg[:], fg_d[:, :], writes=[fg], sbuf=fg)
            iob = kb.sb(pes, "iob", [128, 128], BF16)
            kb.op("dve", lambda e: e.tensor_copy(out=iob[:], in_=iot[:]), [iot], [iob])
            jTb = kb.sb(pes, "jTb", [128, 256], BF16)
            wq = [kb.sb(pes, "wq%d" % i, [128, KC, 128], BF16) for i in range(3)]
            x2s = [kb.sb(pes, "x2s%d" % i, [128, 2, D]) for i in range(2)]
            junk = kb.sb(pes, "junk4", [128, D], BF16)
            ssq = kb.sb(pes, "ssq4", [128, 1])
            rstd = kb.sb(pes, "rstd4", [128, 1])
            ssq2 = kb.sb(pes, "ssq5", [128, 1])
            rstd2 = kb.sb(pes, "rstd5", [128, 1])
            xh = kb.sb(pes, "xh4", [128, D])
            hT = [kb.sb(pes, "hT%d" % i, [128, KC, 256], BF16) for i in range(2)]
            pqT = kb.sb(pes, "pqT", [128, 16, 256], BF16)
            scs = [kb.sb(pes, "scq%d" % i, [128, 2048]) for i in range(2)]
            wk = kb.sb(pes, "wk", [128, 256])
            m16 = kb.sb(pes, "m16", [128, 16, 16])
            i16 = kb.sb(pes, "i16", [128, 16, 16], U32)
            i16f = kb.sb(pes, "i16f", [128, 16, 16])
            cand = kb.sb(pes, "cand", [128, 8, 256])
            t16 = kb.sb(pes, "t16", [128, 8, 16])
            pos = kb.sb(pes, "pos", [128, 8, 16], U32)
            pa = kb.sb(pes, "pa", [128, 8, 16], U32)
            pbb = kb.sb(pes, "pbb", [128, 8, 16], U32)
            paf = kb.sb(pes, "paf", [128, 8, 16])
            pbf = kb.sb(pes, "pbf", [128, 8, 16])
            jl = [kb.sb(pes, "jl%d" % i, [128, 3, 128]) for i in range(2)]
            et = kb.sb(pes, "et", [128, 8, 16])
            es_ = kb.sb(pes, "es_", [128, 8])
            jT = kb.sb(pes, "jT", [128, 3, 256])
            Ub = [kb.sb(pes, "Ub%d" % i, [128, 128], BF16) for i in range(4)]
            Vb4 = [kb.sb(pes, "Vb4%d" % i, [128, 4, 128], BF16) for i in range(2)]
            Gs = kb.sb(pes, "Gs", [128, 256, 128], BF16)
            wd = [kb.sb(pes, "wd%d" % i, [128, 2, KC, 128], BF16) for i in range(3)]
            wu = [kb.sb(pes, "wu%d" % i, [128, 2, D], BF16) for i in range(3)]
            ga = [kb.sb(pes, "ga%d" % i, [128, 256]) for i in range(3)]
            GA = [kb.sb(pes, "GA%d" % i, [128, 256], BF16) for i in range(3)]
            x3s = [kb.sb(pes, "x3_%d" % i, [128, D]) for i in range(2)]
            ssq2s = [kb.sb(pes, "ssq5_%d" % i, [128, 1]) for i in range(2)]
            rstd2s = [kb.sb(pes, "rstd5_%d" % i, [128, 1]) for i in range(2)]
            pacc = [kb.ps(pes, "pacc%d" % i, [128, 512]) for i in range(4)]
            pA = [kb.ps(pes, "pA%d" % i, [128, 512]) for i in range(2)]
            pX = [kb.ps(pes, "pX%d" % i, [128, 512]) for i in range(2)]
            xctr = [0]
            wqc = [0]

            def nx():
                p = pX[xctr[0] % 2]
                xctr[0] += 1
                return p

            def step(gen, n=1):
                if gen is None:
                    return
                for _ in range(n):
                    try:
                        next(gen)
                    except StopIteration:
                        return

            def prea_gen(st):
                sl = st % 2
                for sub in range(2):
                    r = slice(st * 256 + sub * 128, st * 256 + (sub + 1) * 128)
                    kb.dma(x2s[sl][:, sub, :], X2_d[r, :], reads=[dX2], writes=[x2s[sl]], sbuf=x2s[sl])
                    yield
                    kb.op("act", lambda e: e.activation(out=junk[:], in_=x2s[sl][:, sub, :], func=AF.Square, accum_out=ssq[:]),
                          [x2s[sl]], [junk, ssq])
                    kb.op("act", lambda e: e.activation(out=rstd[:], in_=ssq[:], func=AF.Sqrt, bias=EPS, scale=1.0 / D), [ssq], [rstd])
                    kb.op("dve", lambda e: e.reciprocal(out=rstd[:], in_=rstd[:]), [rstd], [rstd])
                    kb.op("act", lambda e: e.activation(out=xh[:], in_=x2s[sl][:, sub, :], func=AF.Copy, scale=rstd[:]),
                          [x2s[sl], rstd], [xh])
                    yield
                    for k2 in range(2):
                        p = nx()
                        for kk in range(4):
                            kc = k2 * 4 + kk
                            kb.op("pe", lambda e: e.transpose(out=p[:, kk * 128:(kk + 1) * 128], in_=xh[:, kc * 128:(kc + 1) * 128],
                                                              identity=identf[:]), [xh, identf], [p])
                        kb.op("dve", lambda e: e.tensor_tensor(out=hT[sl][:, k2 * 4:(k2 + 1) * 4, sub * 128:(sub + 1) * 128],
                                                               in0=p[:].rearrange("p (k t) -> p k t", k=4),
                                                               in1=gains[:, 3, k2 * 4:(k2 + 1) * 4].unsqueeze(2).to_broadcast([128, 4, 128]),
                                                               op=ALU.mult), [p, gains], [hT[sl]])
                        yield
                for hp in range(16):
                    w_ = wq[wqc[0] % 3]
                    wqc[0] += 1
                    kb.dma(w_[:], WPQ_d[:, :, hp * 128:(hp + 1) * 128], reads=[dWPQ], writes=[w_], sbuf=w_)
                    p = nx()
                    for kc in range(KC):
                        kb.op("pe", lambda e: e.matmul(p[:, 0:256], lhsT=w_[:, kc, :], rhs=hT[sl][:, kc, :],
                                                       start=(kc == 0), stop=(kc == KC - 1)), [w_, hT[sl]], [p])
                    kb.op("act", lambda e: e.activation(out=pqT[:, hp, :], in_=p[:, 0:256], func=AF.Copy), [p], [pqT])
                    yield
                for sub in range(2):
                    ts = slice(sub * 128, (sub + 1) * 128)
                    for q4 in range(4):
                        p = nx()
                        for k4 in range(4):
                            hp = q4 * 4 + k4
                            kb.op("pe", lambda e: e.matmul(p[:, k4 * 128:(k4 + 1) * 128], lhsT=pqT[:, hp, ts], rhs=skT[:, hp, :],
                                                           start=True, stop=True), [pqT, skT], [p])
                        kb.op("act", lambda e: e.activation(out=scs[sub][:, q4 * 512:(q4 + 1) * 512], in_=p[:], func=AF.Copy), [p], [scs[sub]])
                        yield

            def topk_gen(st):
                for sub in range(2):
                    scq = scs[sub]
                    jl_ = jl[sub]
                    for hp in range(16):
                        scv = scq[:, hp * 128:(hp + 1) * 128]
                        kb.op("dve", lambda e: e.max(out=m16[:, hp, 0:8], in_=scv), [scq], [m16])
                        kb.op("dve", lambda e: e.max_index(out=i16[:, hp, 0:8], in_max=m16[:, hp, 0:8], in_values=scv), [scq, m16], [i16])
                        yield
                        kb.op("dve", lambda e: e.match_replace(out=wk[:, 0:128], in_to_replace=m16[:, hp, 0:8], in_values=scv,
                                                               imm_value=-1e30), [scq, m16], [wk])
                        kb.op("dve", lambda e: e.max(out=m16[:, hp, 8:16], in_=wk[:, 0:128]), [wk], [m16])
                        kb.op("dve", lambda e: e.max_index(out=i16[:, hp, 8:16], in_max=m16[:, hp, 8:16], in_values=wk[:, 0:128]), [wk, m16], [i16])
                        yield
                    kb.op("dve", lambda e: e.tensor_copy(out=i16f[:], in_=i16[:]), [i16], [i16f])
                    m16v = m16[:].rearrange("p (h s) k -> p h s k", s=2)
                    i16v = i16f[:].rearrange("p (h s) k -> p h s k", s=2)
                    yield
                    kb.op("dve", lambda e: e.tensor_tensor(out=cand[:].rearrange("p h (a b) -> p h a b", b=16),
                                                           in0=m16v[:, :, 0, :].unsqueeze(3).to_broadcast([128, 8, 16, 16]),
                                                           in1=m16v[:, :, 1, :].unsqueeze(2).to_broadcast([128, 8, 16, 16]), op=ALU.add),
                          [m16], [cand])
                    yield
                    for h in range(8):
                        kb.op("dve", lambda e: e.max(out=t16[:, h, 0:8], in_=cand[:, h, :]), [cand], [t16])
                        kb.op("dve", lambda e: e.max_index(out=pos[:, h, 0:8], in_max=t16[:, h, 0:8], in_values=cand[:, h, :]), [cand, t16], [pos])
                        yield
                        kb.op("dve", lambda e: e.match_replace(out=wk[:], in_to_replace=t16[:, h, 0:8], in_values=cand[:, h, :],
                                                               imm_value=-1e30), [cand, t16], [wk])
                        kb.op("dve", lambda e: e.max(out=t16[:, h, 8:16], in_=wk[:]), [wk], [t16])
                        kb.op("dve", lambda e: e.max_index(out=pos[:, h, 8:16], in_max=t16[:, h, 8:16], in_values=wk[:]), [wk, t16], [pos])
                        yield
                    kb.op("dve", lambda e: e.tensor_tensor(out=et[:], in0=t16[:], in1=t16[:, :, 0:1].to_broadcast([128, 8, 16]),
                                                           op=ALU.subtract), [t16], [et])
                    kb.op("dve", lambda e: e.tensor_single_scalar(out=pa[:], in_=pos[:], scalar=4, op=ALU.logical_shift_right), [pos], [pa])
                    kb.op("dve", lambda e: e.tensor_single_scalar(out=pbb[:], in_=pos[:], scalar=15, op=ALU.bitwise_and), [pos], [pbb])
                    yield
                    kb.op("dve", lambda e: e.tensor_copy(out=paf[:], in_=pa[:]), [pa], [paf])
                    kb.op("dve", lambda e: e.tensor_copy(out=pbf[:], in_=pbb[:]), [pbb], [pbf])
                    yield
                    kb.op("act", lambda e: e.activation(out=et[:], in_=et[:], func=AF.Exp), [et], [et])
                    eqv = scq[:].rearrange("p (h k a) -> p h k a", h=8, k=16)
                    io16 = iot[:, 0:16].unsqueeze(1).unsqueeze(1).to_broadcast([128, 8, 16, 16])
                    for side, pf in ((0, paf), (1, pbf)):
                        kb.op("dve", lambda e: e.tensor_tensor(out=eqv, in0=pf[:].unsqueeze(3).to_broadcast([128, 8, 16, 16]), in1=io16,
                                                               op=ALU.is_equal), [pf, iot], [scq])
                        yield
                        kb.op("dve", lambda e: e.tensor_tensor(out=eqv, in0=eqv,
                                                               in1=i16v[:, :, side, :].unsqueeze(2).to_broadcast([128, 8, 16, 16]),
                                                               op=ALU.mult), [scq, i16f], [scq])
                        yield
                        kb.op("dve", lambda e: e.tensor_reduce(out=jl_[:, side, :].rearrange("p (h k) -> p h k", h=8), in_=eqv,
                                                               axis=AX.X, op=ALU.add), [scq], [jl_])
                        yield
                    kb.op("dve", lambda e: e.tensor_reduce(out=es_[:], in_=et[:], axis=AX.X, op=ALU.add), [et], [es_])
                    kb.op("dve", lambda e: e.reciprocal(out=es_[:], in_=es_[:]), [es_], [es_])
                    kb.op("dve", lambda e: e.tensor_tensor(out=jl_[:, 2, :].rearrange("p (h k) -> p h k", h=8), in0=et[:],
                                                           in1=es_[:].unsqueeze(2).to_broadcast([128, 8, 16]), op=ALU.mult), [et, es_], [jl_])
                    yield

            def jt_emit():
                for sub in range(2):
                    ts = slice(sub * 128, (sub + 1) * 128)
                    p = nx()
                    for a in range(3):
                        kb.op("pe", lambda e: e.transpose(out=p[:, a * 128:(a + 1) * 128], in_=jl[sub][:, a, :], identity=identf[:]),
                              [jl[sub], identf], [p])
                    kb.op("act", lambda e: e.activation(out=jT[:, :, ts], in_=p[:, 0:384].rearrange("p (a t) -> p a t", a=3), func=AF.Copy),
                          [p], [jT])
                    kb.op("act", lambda e: e.activation(out=jTb[:, ts], in_=p[:, 128:256], func=AF.Copy), [p], [jTb])

            def ld(g):
                k = g % 3
                kb.dma(wd[k][:].rearrange("p a b c -> p (a b c)"), WD_d[g], reads=[dWD[g]], writes=[wd[k]], sbuf=wd[k])
                kb.dma(wu[k][:].rearrange("p a d -> p (a d)"), WU_d[g], reads=[dWU[g]], writes=[wu[k]], sbuf=wu[k])

            pending_post = []

            def post_b(st_):
                for sub in range(2):
                    x3 = x3s[sub]
                    kb.op("dve", lambda e: e.reciprocal(out=rstd2s[sub][:], in_=rstd2s[sub][:]), [rstd2s[sub]], [rstd2s[sub]])
                    kb.op("dve", lambda e: e.scalar_tensor_tensor(out=x3[:], in0=x3[:], scalar=rstd2s[sub][:, 0:1], in1=fg[:], op0=ALU.mult,
                                                                  op1=ALU.mult), [x3, rstd2s[sub], fg], [x3])
                    r = slice(st_ * 256 + sub * 128, st_ * 256 + (sub + 1) * 128)
                    kb.dma(out_d[r, :], x3[:], reads=[x3], writes=[], sbuf=x3)

            step(prea_gen(0), 100000)
            step(topk_gen(0), 100000)
            jt_emit()
            for st in range(NST):
                sl = st % 2
                hT_ = hT[sl]
                prea = prea_gen(st + 1) if st + 1 < NST else None
                topk = topk_gen(st + 1) if st + 1 < NST else None
                ld(0)
                ld(1)
                for t0 in range(0, 256, 4):
                    p = nx()
                    for tt in range(4):
                        t = t0 + tt
                        ub = Ub[t % 4]
                        if tt == 0:
                            vb4 = Vb4[(t0 // 4) % 2]
                            kb.op("dve", lambda e: e.tensor_tensor(out=vb4[:], in0=iob[:].unsqueeze(1).to_broadcast([128, 4, 128]),
                                                                   in1=jTb[:, t0:t0 + 4].unsqueeze(2).to_broadcast([128, 4, 128]),
                                                                   op=ALU.is_equal), [iob, jTb], [vb4])
                        kb.op("dve", lambda e: e.tensor_scalar(out=ub[:], in0=iob[:], scalar1=jT[:, 0, t:t + 1], scalar2=jT[:, 2, t:t + 1],
                                                               op0=ALU.is_equal, op1=ALU.mult), [iob, jT], [ub])
                        kb.op("pe", lambda e: e.matmul(p[:, tt * 128:(tt + 1) * 128], lhsT=ub[:], rhs=vb4[:, tt, :], start=True, stop=True),
                              [ub, vb4], [p])
                    kb.op("act", lambda e: e.activation(out=Gs[:, t0:t0 + 4, :], in_=p[:].rearrange("p (t n) -> p t n", t=4), func=AF.Copy),
                          [p], [Gs])
                    step(prea, 1)
                    if t0 == 16 and pending_post:
                        post_b(pending_post.pop(0))
                step(prea, 100000)

                def down(n2):
                    g, q = divmod(n2, 2)
                    k = g % 3
                    pa_ = pA[n2 % 2]
                    for kc in range(KC):
                        kb.op("pe", lambda e: e.matmul(pa_[:, 0:256], lhsT=wd[k][:, q, kc, :], rhs=hT_[:, kc, :], start=(kc == 0),
                                                       stop=(kc == KC - 1)), [wd[k], hT_], [pa_])

                down(0)
                for n2 in range(128):
                    g, q = divmod(n2, 2)
                    k = g % 3
                    if q == 0 and g + 2 < 64:
                        ld(g + 2)
                    if n2 + 1 < 128:
                        down(n2 + 1)
                    pa_ = pA[n2 % 2]
                    gk = ga[n2 % 3]
                    Gk = GA[n2 % 3]
                    kb.op("act", lambda e: e.activation(out=gk[:], in_=pa_[:, 0:256], func=GELU), [pa_], [gk])
                    kb.op("dve", lambda e: e.tensor_tensor(out=Gk[:], in0=gk[:], in1=Gs[:, :, n2], op=ALU.mult), [gk, Gs], [Gk])
                    for sub in range(2):
                        for half in range(2):
                            kb.op("pe", lambda e: e.matmul(pacc[sub * 2 + half][:], lhsT=Gk[:, sub * 128:(sub + 1) * 128],
                                                           rhs=wu[k][:, q, half * 512:(half + 1) * 512], start=(n2 == 0), stop=(n2 == 127)),
                                  [Gk, wu[k]], [pacc[sub * 2 + half]])
                    step(topk, 1)
                step(topk, 100000)
                if st + 1 < NST:
                    jt_emit()
                for sub in range(2):
                    x3 = x3s[sub]
                    for half in range(2):
                        hs = slice(half * 512, (half + 1) * 512)
                        kb.op("dve", lambda e: e.tensor_tensor(out=x3[:, hs], in0=pacc[sub * 2 + half][:], in1=x2s[sl][:, sub, hs], op=ALU.add),
                              [pacc[sub * 2 + half], x2s[sl]], [x3])
                    kb.op("act", lambda e: e.activation(out=junk[:], in_=x3[:], func=AF.Square, accum_out=ssq2s[sub][:]), [x3], [junk, ssq2s[sub]])
                for sub in range(2):
                    kb.op("act", lambda e: e.activation(out=rstd2s[sub][:], in_=ssq2s[sub][:], func=AF.Sqrt, bias=EPS, scale=1.0 / D),
                          [ssq2s[sub]], [rstd2s[sub]])
                pending_post.append(st)
            post_b(pending_post.pop(0))
            kb.barrier()

    if stop_after >= 1:
        phase1()
    if stop_after >= 2:
        phase2()
    if stop_after >= 3:
        phase3a()
    if stop_after >= 4:
        phase3b()
    kb.barrier(("sp",))
    es.close()
    return nc, kb


def host_consts(S):
    NT = S // 128
    ident = np.eye(128, dtype=np.float32)
    k = np.arange(128)
    tri = (k[:, None] <= k[None, :]).astype(np.float32)
    iota = np.tile(np.arange(128, dtype=np.float32)[None, :], (128, 1))
    half = 32
    freqs = (10000.0 ** (-np.arange(half, dtype=np.float32) / half)).astype(np.float32)
    pos = np.arange(S, dtype=np.float32)
    ang = (pos[:, None] * freqs[None, :]).astype(np.float32)
    cos = np.cos(ang).astype(np.float32).reshape(NT, 128, 32).transpose(1, 0, 2)
    sin = np.sin(ang).astype(np.float32).reshape(NT, 128, 32).transpose(1, 0, 2)
    ind = np.zeros((32, S), dtype=np.float32)
    for n in range(S // 256):
        ind[n, n * 256:(n + 1) * 256] = 1.0
    pastneg = np.zeros((NT, 32), dtype=np.float32)
    ownind = np.zeros((NT, 32), dtype=np.float32)
    for c in range(NT):
        own = c // 2
        pastneg[c, own:] = -1e30
        ownind[c, own] = 1.0
    return {
        "ident": ident, "tri": tri, "iota": iota,
        "cos": np.ascontiguousarray(cos), "sin": np.ascontiguousarray(sin),
        "ind": ind.astype(ml_dtypes.bfloat16),
        "pastneg": np.ascontiguousarray(np.broadcast_to(pastneg[None], (128, NT, 32))),
        "ownind": np.ascontiguousarray(np.broadcast_to(ownind[None], (128, NT, 32))),
    }


def host_weights(norm_mix_g, w_in, moba_w_proj, gmlp_norm_g, gmlp_w_s, gmlp_b_s, gmlp_w_proj, w_out, norm_xa_g,
                 norm_mem_g, xa_w_q, xa_w_kv, xa_w_o, norm_ffn_g, peer_w_q, peer_subkeys, peer_w_down, peer_w_up, final_g):
    f = lambda a: np.ascontiguousarray(np.asarray(a, dtype=np.float32))
    gains = np.stack([np.asarray(g, np.float32)[0].reshape(8, 128).T for g in (norm_mix_g, norm_xa_g, norm_mem_g, norm_ffn_g)], axis=1)
    wd = np.asarray(peer_w_down, np.float32)[0].reshape(128, 128, 8, 128)
    wdT = np.ascontiguousarray(wd.transpose(3, 1, 2, 0)).reshape(128, 131072)
    wu = np.asarray(peer_w_up, np.float32)[0].reshape(128, 131072)
    sk = np.asarray(peer_subkeys, np.float32)[0].reshape(16, 128, 128)
    return {
        "w_in": f(w_in[0]), "gproj": f(gmlp_w_proj[0]), "mproj": f(moba_w_proj[0]), "w_out": f(w_out[0]),
        "xa_wq": f(xa_w_q[0]), "xa_wkv": f(xa_w_kv[0]), "xa_wo": f(xa_w_o[0]), "peer_wq": f(peer_w_q[0]),
        "gains": f(gains),
        "gng": f(np.broadcast_to(np.asarray(gmlp_norm_g, np.float32)[0][None, :], (128, 512))),
        "bs": f(np.asarray(gmlp_b_s, np.float32)[0].T),
        "fg": f(np.broadcast_to(np.asarray(final_g, np.float32)[None, :], (128, D))),
        "wmT": f(np.asarray(gmlp_w_s, np.float32)[0].transpose(2, 0, 1)),
        "skT": f(sk.transpose(2, 0, 1)),
        "wdT": wdT, "wu": f(wu),
    }


_CACHE = {}


def kernel(x, mem, **w):
    x = np.asarray(x, np.float32)
    mem = np.asarray(mem, np.float32)
    B, S, _ = x.shape
    if S not in _CACHE:
        _CACHE[S] = build(S)[0]
    nc = _CACHE[S]
    shared = dict(host_consts(S))
    shared.update(host_weights(**w))
    in_maps = []
    for b in range(B):
        m = dict(shared)
        m["x"] = np.ascontiguousarray(x[b])
        m["mem"] = np.ascontiguousarray(mem[b])
        in_maps.append(m)
    res = run_bass_kernel_spmd(nc, in_maps, core_ids=list(range(B)))
    return np.stack([np.asarray(r["out"], np.float32) for r in res.results], axis=0)
```

```python
import contextlib
import os
import numpy as np
import ml_dtypes
import concourse.bass as bass
import concourse.mybir as mybir
from concourse.bass_utils import run_bass_kernel_spmd

F32 = mybir.dt.float32
BF16 = mybir.dt.bfloat16
U32 = mybir.dt.uint32
AF = mybir.ActivationFunctionType
ALU = mybir.AluOpType
AX = mybir.AxisListType
D = 1024
KC = 8
EPS = 1e-6
GELU = AF.Gelu_apprx_tanh
NEGM = -30000.0


class Buf:
    __slots__ = ("w", "r", "sem", "dc", "name")

    def __init__(self, name=""):
        self.w = None
        self.r = {}
        self.sem = None
        self.dc = 0
        self.name = name


class T:
    def __init__(self, t, name):
        self.t = t
        self.b = Buf(name)

    def __getitem__(self, k):
        return self.t[k]


class KB:
    def __init__(self, nc, es):
        self.nc = nc
        self.es = es
        self.E = {"pe": nc.tensor, "act": nc.scalar, "dve": nc.vector, "pool": nc.gpsimd, "sp": nc.sync}
        self.esem = {}
        self.ecnt = {}
        for e in ("pe", "act", "dve", "pool"):
            self.esem[e] = es.enter_context(nc.semaphore("sem_" + e))
            self.ecnt[e] = 0
        self.seen = {e: {} for e in self.E}
        self.dbufs = []
        self.n = 0

    def sb(self, es, name, shape, dt=F32):
        return T(es.enter_context(self.nc.sbuf_tensor("s_" + name, shape, dt)), name)

    def ps(self, es, name, shape, dt=F32):
        return T(es.enter_context(self.nc.psum_tensor("p_" + name, shape, dt)), name)

    def _wait(self, e, rec):
        sem, val = rec
        k = id(sem)
        if self.seen[e].get(k, 0) >= val:
            return
        self.E[e].wait_ge(sem, val)
        self.seen[e][k] = val

    def _dep(self, e, rec):
        if e == "pe" and rec[0] is self.esem["pe"]:
            return
        self._wait(e, rec)

    def _deps(self, e, reads, writes):
        own = self.esem.get(e)
        for b in reads:
            if b.w is not None:
                self._dep(e, b.w)
        for b in writes:
            if b.w is not None and b.w[0] is not own:
                self._dep(e, b.w)
            for rec in b.r.values():
                if rec[0] is not own:
                    self._dep(e, rec)

    def op(self, e, fn, reads=(), writes=()):
        reads = [x.b if isinstance(x, T) else x for x in reads]
        writes = [x.b if isinstance(x, T) else x for x in writes]
        self._deps(e, reads, writes)
        ins = fn(self.E[e])
        self.ecnt[e] += 1
        ins.then_inc(self.esem[e], 1)
        rec = (self.esem[e], self.ecnt[e])
        for b in reads:
            b.r[id(rec[0])] = rec
        for b in writes:
            b.w = rec
            b.r = {}
        self.n += 1
        return ins

    def dma(self, out, in_, reads=(), writes=(), sbuf=None, q=None):
        if q is None:
            q = "pool" if str(out.space) == "DRAM" else "sp"
        reads = [x.b if isinstance(x, T) else x for x in reads]
        writes = [x.b if isinstance(x, T) else x for x in writes]
        b = sbuf.b if isinstance(sbuf, T) else sbuf
        if b.sem is None:
            b.sem = self.es.enter_context(self.nc.semaphore("dsem%d" % len(self.dbufs)))
            self.dbufs.append(b)
        self._deps(q, reads, writes)
        if b.dc > 0:
            self._wait(q, (b.sem, b.dc))
        ins = self.E[q].dma_start(out=out, in_=in_)
        b.dc += 16
        ins.then_inc(b.sem, 16)
        rec = (b.sem, b.dc)
        for x in reads:
            x.r[id(rec[0])] = rec
        for x in writes:
            x.w = rec
            x.r = {}
        self.n += 1
        return ins

    def barrier(self, engines=("pe", "act", "dve", "pool", "sp")):
        for e in engines:
            for s in ("pe", "act", "dve", "pool"):
                if self.ecnt[s] > 0 and not (e == s):
                    self._wait(e, (self.esem[s], self.ecnt[s]))
            for b in self.dbufs:
                if b.dc > 0:
                    self._wait(e, (b.sem, b.dc))


def build(S, dbg=False, stop_after=9):
    NT = S // 128
    NB = S // 256
    NST = S // 256
    nc = bass.Bass("TRN2", target_bir_lowering=False)
    es = contextlib.ExitStack()
    kb = KB(nc, es)

    def din(name, shape, dt=F32):
        return nc.dram_tensor(name, shape, dt, kind="ExternalInput").ap()

    def dscr(name, shape, dt):
        if dbg:
            return nc.dram_tensor(name, shape, dt, kind="ExternalOutput").ap()
        return nc.dram_tensor(name, shape, dt).ap()

    x_d = din("x", [S, D])
    mem_d = din("mem", [256, D])
    w_in_d = din("w_in", [D, 4608])
    gproj_d = din("gproj", [512, D])
    mproj_d = din("mproj", [512, D])
    wout_d = din("w_out", [D, D])
    xwq_d = din("xa_wq", [D, 512])
    xwkv_d = din("xa_wkv", [D, D])
    xwo_d = din("xa_wo", [512, D])
    pwq_d = din("peer_wq", [D, 2048])
    gains_d = din("gains", [128, 4, 8])
    gng_d = din("gng", [128, 512])
    bs_d = din("bs", [128, 8])
    fg_d = din("fg", [128, D])
    wmT_d = din("wmT", [128, 8, 128])
    skT_d = din("skT", [128, 16, 128])
    wdT_d = din("wdT", [128, 131072])
    wu_d = din("wu", [128, 131072])
    ident_d = din("ident", [128, 128])
    tri_d = din("tri", [128, 128])
    iota_d = din("iota", [128, 128])
    cos_d = din("cos", [128, NT, 32])
    sin_d = din("sin", [128, NT, 32])
    ind_d = din("ind", [32, S], BF16)
    pastneg_d = din("pastneg", [128, NT, 32])
    ownind_d = din("ownind", [128, NT, 32])
    out_d = nc.dram_tensor("out", [S, D], F32, kind="ExternalOutput").ap()

    QT_d = dscr("QT", [512, S], BF16)
    KT_d = dscr("KT", [512, S], BF16)
    VS_d = dscr("VS", [NT, 128, 8, 65], BF16)
    SIGA_d = dscr("SIGA", [S, D], BF16)
    MB_d = dscr("MB", [S, D], F32)
    ATT_d = dscr("ATT", [S, 512], BF16)
    X2_d = dscr("X2", [S, D], F32)
    WD_d = dscr("WD", [64, 128, 2048], BF16)
    WU_d = dscr("WU", [64, 128, 2048], BF16)
    dQT, dKT, dVS, dSIGA, dMB, dATT, dX2 = (Buf(n) for n in ("QT", "KT", "VS", "SIGA", "MB", "ATT", "X2"))
    dWD = [Buf("WD%d" % i) for i in range(64)]
    dWU = [Buf("WU%d" % i) for i in range(64)]
    DR = Buf("dram_const")

    identf = kb.sb(es, "identf", [128, 128])
    identb = kb.sb(es, "identb", [128, 128], BF16)
    trib = kb.sb(es, "trib", [128, 128], BF16)
    trif = kb.sb(es, "trif", [128, 128])
    gains = kb.sb(es, "gains", [128, 4, 8])
    qsq = kb.sb(es, "qsq", [128, NT, 8])
    kmax2 = kb.sb(es, "kmax2", [128, 8])
    negshift = kb.sb(es, "negshift", [128, NT, 8])
    stg = [None, None]
    cvo = [None, None]

    def alloc_stg(pes, with_cvo=False):
        for i in range(2):
            stg[i] = kb.sb(pes, "stg%d_%d" % (i, kb.n), [128, 2048])
            if with_cvo:
                cvo[i] = kb.sb(pes, "cvo%d_%d" % (i, kb.n), [128, 2048], BF16)

    kb.dma(identf[:], ident_d[:, :], writes=[identf], sbuf=identf)
    kb.dma(trif[:], tri_d[:, :], writes=[trif], sbuf=trif)
    kb.dma(gains[:], gains_d[:, :, :], writes=[gains], sbuf=gains)
    kb.op("dve", lambda e: e.tensor_copy(out=identb[:], in_=identf[:]), [identf], [identb])
    kb.op("dve", lambda e: e.tensor_copy(out=trib[:], in_=trif[:]), [trif], [trib])
    kb.op("dve", lambda e: e.memset(kmax2[:], 0.0), [], [kmax2])

    cast_ctr = [0]

    def cast(out_ap, in_ap, reads, writes, eng=None):
        if eng is None:
            eng = ("act", "dve")[cast_ctr[0] % 2]
            cast_ctr[0] += 1
        if eng == "act":
            kb.op("act", lambda e: e.activation(out=out_ap, in_=in_ap, func=AF.Copy), reads, writes)
        else:
            kb.op(eng, lambda e: e.tensor_copy(out=out_ap, in_=in_ap), reads, writes)

    def load_w(dst, src_d, K, N, scale=None):
        for kc in range(K // 128):
            for c0 in range(0, N, 2048):
                w = min(2048, N - c0)
                s = stg[cast_ctr[0] % 2]
                kb.dma(s[:, 0:w], src_d[kc * 128:(kc + 1) * 128, c0:c0 + w], writes=[s], sbuf=s)
                if scale is None:
                    cast(dst[:, kc, c0:c0 + w], s[:, 0:w], [s], [dst])
                else:
                    cast_ctr[0] += 1
                    kb.op("dve", lambda e: e.tensor_single_scalar(out=dst[:, kc, c0:c0 + w], in_=s[:, 0:w], scalar=scale, op=ALU.mult),
                          [s], [dst])

    def conv_gen():
        for which, (src, dst, dbl) in enumerate(((wdT_d, WD_d, dWD), (wu_d, WU_d, dWU))):
            for c in range(64):
                k = c % 2
                s = stg[k]
                o = cvo[k]
                kb.dma(s[:], src[:, c * 2048:(c + 1) * 2048], writes=[s], sbuf=s)
                yield
                kb.op("dve", lambda e: e.tensor_copy(out=o[:], in_=s[:]), [s], [o])
                kb.dma(dst[c], o[:], reads=[o], writes=[dbl[c]], sbuf=o)
                yield

    def phase1():
        with contextlib.ExitStack() as pes:
            alloc_stg(pes)
            w_in = kb.sb(pes, "w_in_sb", [128, KC, 4608], BF16)
            gproj = kb.sb(pes, "gproj_sb", [128, 4, D], BF16)
            wmT = kb.sb(pes, "wmT_sb", [128, 8, 128], BF16)
            wmTf = kb.sb(pes, "wmTf", [128, 8, 128])
            cos = kb.sb(pes, "cos_sb", [128, NT, 32])
            sin = kb.sb(pes, "sin_sb", [128, NT, 32])
            gng = kb.sb(pes, "gng_sb", [128, 512])
            bs = kb.sb(pes, "bs_sb", [128, 8])
            kb.dma(cos[:], cos_d[:, :, :], writes=[cos], sbuf=cos)
            kb.dma(sin[:], sin_d[:, :, :], writes=[sin], sbuf=sin)
            kb.dma(gng[:], gng_d[:, :], writes=[gng], sbuf=gng)
            kb.dma(bs[:], bs_d[:, :], writes=[bs], sbuf=bs)
            kb.dma(wmTf[:], wmT_d[:, :, :], writes=[wmTf], sbuf=wmTf)
            kb.op("dve", lambda e: e.tensor_tensor(out=wmT[:], in0=wmTf[:],
                                                   in1=trif[:].unsqueeze(1).to_broadcast([128, 8, 128]),
                                                   op=ALU.mult), [wmTf, trif], [wmT])
            load_w(w_in, w_in_d, D, 4608)
            load_w(gproj, gproj_d, 512, D, scale=0.5)

            xs = [kb.sb(pes, "xs%d" % i, [128, D]) for i in range(3)]
            junk = kb.sb(pes, "junk", [128, D], BF16)
            ssq = [kb.sb(pes, "ssq%d" % i, [128, 1]) for i in range(2)]
            rstd = [kb.sb(pes, "rstd%d" % i, [128, 1]) for i in range(2)]
            xh = [kb.sb(pes, "xh%d" % i, [128, D], BF16) for i in range(2)]
            xT = [kb.sb(pes, "xT%d" % i, [128, KC, 128], BF16) for i in range(2)]
            t1 = kb.sb(pes, "t1", [128, 8, 32])
            t2 = kb.sb(pes, "t2", [128, 8, 32])
            sqt = kb.sb(pes, "sqt", [128, 8, 64])
            ksq = kb.sb(pes, "ksq", [128, 8])
            qr = [kb.sb(pes, "qr%d" % i, [128, 8, 64], BF16) for i in range(2)]
            kr = [kb.sb(pes, "kr%d" % i, [128, 8, 64], BF16) for i in range(2)]
            va = [kb.sb(pes, "va%d" % i, [128, 8, 65], BF16) for i in range(2)]
            u = [kb.sb(pes, "u%d" % i, [128, 512]) for i in range(2)]
            gv = kb.sb(pes, "gv", [128, 512])
            ssv = kb.sb(pes, "ssv", [128, 1])
            rsv = kb.sb(pes, "rsv", [128, 1])
            vh = [kb.sb(pes, "vh%d" % i, [128, 512], BF16) for i in range(2)]
            siga = [kb.sb(pes, "siga%d" % i, [128, D], BF16) for i in range(2)]
            sigb = [kb.sb(pes, "sigb%d" % i, [128, D]) for i in range(2)]
            qkT = [kb.sb(pes, "qkT%d" % i, [128, 8, 128], BF16) for i in range(2)]
            z1 = kb.sb(pes, "z1", [128, 512])
            sg = [kb.sb(pes, "sg%d" % i, [128, 512], BF16) for i in range(2)]
            sgT = [kb.sb(pes, "sgT%d" % i, [128, 4, 128], BF16) for i in range(2)]
            mb = [kb.sb(pes, "mb%d" % i, [128, D]) for i in range(2)]
            for i in range(2):
                kb.op("dve", lambda e: e.memset(va[i][:], 1.0), [], [va[i]])

            pT = kb.ps(pes, "pT", [128, 1024], BF16)
            pQK = kb.ps(pes, "pQK", [128, 1024], BF16)
            pZ = kb.ps(pes, "pZ", [128, 512])
            pST = kb.ps(pes, "pST", [128, 1024], BF16)
            pP = [kb.ps(pes, "pP%d" % i, [128, 512]) for i in range(4)]
            pctr = [0]

            def next_p():
                p = pP[pctr[0] % 4]
                pctr[0] += 1
                return p

            def pre_load(i):
                kb.dma(xs[i % 3][:], x_d[i * 128:(i + 1) * 128, :], reads=[DR], writes=[xs[i % 3]], sbuf=xs[i % 3])

            def pre_sq(i):
                s = i % 2
                kb.op("act", lambda e: e.activation(out=junk[:], in_=xs[i % 3][:], func=AF.Square, accum_out=ssq[s][:]),
                      [xs[i % 3]], [junk, ssq[s]])

            def pre_sqrt(i):
                s = i % 2
                kb.op("act", lambda e: e.activation(out=rstd[s][:], in_=ssq[s][:], func=AF.Sqrt, bias=EPS, scale=1.0 / D),
                      [ssq[s]], [rstd[s]])
                kb.op("dve", lambda e: e.reciprocal(out=rstd[s][:], in_=rstd[s][:]), [rstd[s]], [rstd[s]])

            def pre_xh(i):
                s = i % 2
                kb.op("act", lambda e: e.activation(out=xh[s][:], in_=xs[i % 3][:], func=AF.Copy, scale=rstd[s][:]),
                      [xs[i % 3], rstd[s]], [xh[s]])

            def pre_b(i):
                s = i % 2
                for kc in range(KC):
                    kb.op("pe", lambda e: e.transpose(out=pT[:, kc * 128:(kc + 1) * 128], in_=xh[s][:, kc * 128:(kc + 1) * 128],
                                                      identity=identb[:]), [xh[s], identb], [pT])
                kb.op("dve", lambda e: e.tensor_tensor(out=xT[s][:], in0=pT[:].rearrange("p (k t) -> p k t", k=KC),
                                                       in1=gains[:, 0, :].unsqueeze(2).to_broadcast([128, KC, 128]),
                                                       op=ALU.mult), [pT, gains], [xT[s]])

            def mm_group(i, n):
                s = i % 2
                p = next_p()
                for kc in range(KC):
                    kb.op("pe", lambda e: e.matmul(p[:], lhsT=xT[s][:, kc, :], rhs=w_in[:, kc, n * 512:(n + 1) * 512],
                                                   start=(kc == 0), stop=(kc == KC - 1)), [xT[s], w_in], [p])
                return p

            def rope(p, dst, i):
                pv = p[:].rearrange("p (h d) -> p h d", h=8)
                x1 = pv[:, :, 0:32]
                x2 = pv[:, :, 32:64]
                cb = cos[:, i, :].unsqueeze(1).to_broadcast([128, 8, 32])
                sbb = sin[:, i, :].unsqueeze(1).to_broadcast([128, 8, 32])
                kb.op("dve", lambda e: e.tensor_tensor(out=t1[:], in0=x1, in1=cb, op=ALU.mult), [p, cos], [t1])
                kb.op("dve", lambda e: e.tensor_tensor(out=t2[:], in0=x2, in1=sbb, op=ALU.mult), [p, sin], [t2])
                kb.op("dve", lambda e: e.tensor_tensor(out=dst[:, :, 0:32], in0=t1[:], in1=t2[:], op=ALU.subtract), [t1, t2], [dst])
                kb.op("dve", lambda e: e.tensor_tensor(out=t1[:], in0=x2, in1=cb, op=ALU.mult), [p, cos], [t1])
                kb.op("dve", lambda e: e.tensor_tensor(out=t2[:], in0=x1, in1=sbb, op=ALU.mult), [p, sin], [t2])
                kb.op("dve", lambda e: e.tensor_tensor(out=dst[:, :, 32:64], in0=t1[:], in1=t2[:], op=ALU.add), [t1, t2], [dst])

            def tail2(i):
                s = i % 2
                for j in range(4):
                    kb.op("pe", lambda e: e.transpose(out=pST[:, j * 128:(j + 1) * 128], in_=sg[s][:, j * 128:(j + 1) * 128],
                                                      identity=identb[:]), [sg[s], identb], [pST])
                kb.op("act", lambda e: e.activation(out=sgT[s][:], in_=pST[:, 0:512].rearrange("p (k t) -> p k t", k=4), func=AF.Copy),
                      [pST], [sgT[s]])
                for half in range(2):
                    p = next_p()
                    for kc in range(4):
                        kb.op("pe", lambda e: e.matmul(p[:], lhsT=sgT[s][:, kc, :], rhs=gproj[:, kc, half * 512:(half + 1) * 512],
                                                       start=(kc == 0), stop=(kc == 3)), [sgT[s], gproj], [p])
                    kb.op("dve", lambda e: e.scalar_tensor_tensor(out=mb[s][:, half * 512:(half + 1) * 512],
                                                                  in0=sigb[s][:, half * 512:(half + 1) * 512], scalar=1.0, in1=p[:],
                                                                  op0=ALU.add, op1=ALU.mult), [p, sigb[s]], [mb[s]])
                kb.dma(MB_d[i * 128:(i + 1) * 128, :], mb[s][:], reads=[mb[s]], writes=[dMB], sbuf=mb[s])

            pre_load(0)
            if NT > 1:
                pre_load(1)
            pre_sq(0)
            pre_sqrt(0)
            pre_xh(0)
            pre_b(0)
            for i in range(NT):
                s = i % 2
                if i + 2 < NT:
                    pre_load(i + 2)
                p = mm_group(i, 0)
                kb.op("act", lambda e: e.activation(out=sqt[:], in_=p[:].rearrange("p (h d) -> p h d", h=8), func=AF.Square), [p], [sqt])
                kb.op("dve", lambda e: e.tensor_reduce(out=qsq[:, i, :], in_=sqt[:], axis=AX.X, op=ALU.add), [sqt], [qsq])
                rope(p, qr[s], i)
                p = mm_group(i, 1)
                kb.op("act", lambda e: e.activation(out=sqt[:], in_=p[:].rearrange("p (h d) -> p h d", h=8), func=AF.Square), [p], [sqt])
                kb.op("dve", lambda e: e.tensor_reduce(out=ksq[:], in_=sqt[:], axis=AX.X, op=ALU.add), [sqt], [ksq])
                kb.op("dve", lambda e: e.tensor_tensor(out=kmax2[:], in0=kmax2[:], in1=ksq[:], op=ALU.max), [kmax2, ksq], [kmax2])
                rope(p, kr[s], i)
                p = mm_group(i, 2)
                kb.op("act", lambda e: e.activation(out=va[s][:, :, 0:64], in_=p[:].rearrange("p (h d) -> p h d", h=8), func=AF.Copy),
                      [p], [va[s]])
                kb.dma(VS_d[i], va[s][:], reads=[va[s]], writes=[dVS], sbuf=va[s])
                p = mm_group(i, 3)
                kb.op("act", lambda e: e.activation(out=u[s][:], in_=p[:], func=GELU), [p], [u[s]])
                if i > 0:
                    tail2(i - 1)
                p = mm_group(i, 4)
                kb.op("act", lambda e: e.activation(out=gv[:], in_=p[:], func=GELU), [p], [gv])
                kb.op("act", lambda e: e.activation(out=junk[:, 0:512], in_=gv[:], func=AF.Square, accum_out=ssv[:]), [gv], [junk, ssv])
                if i + 1 < NT:
                    pre_sq(i + 1)
                kb.op("act", lambda e: e.activation(out=rsv[:], in_=ssv[:], func=AF.Sqrt, bias=EPS, scale=1.0 / 512), [ssv], [rsv])
                if i + 1 < NT:
                    pre_sqrt(i + 1)
                kb.op("dve", lambda e: e.reciprocal(out=rsv[:], in_=rsv[:]), [rsv], [rsv])
                kb.op("act", lambda e: e.activation(out=vh[s][:], in_=gv[:], func=AF.Copy, scale=rsv[:]), [gv, rsv], [vh[s]])
                if i + 1 < NT:
                    pre_xh(i + 1)
                for n in (5, 6):
                    p = mm_group(i, n)
                    kb.op("act", lambda e: e.activation(out=siga[s][:, (n - 5) * 512:(n - 4) * 512], in_=p[:], func=AF.Tanh, scale=0.5),
                          [p], [siga[s]])
                kb.dma(SIGA_d[i * 128:(i + 1) * 128, :], siga[s][:], reads=[siga[s]], writes=[dSIGA], sbuf=siga[s])
                if i + 1 < NT:
                    pre_b(i + 1)
                for n in (7, 8):
                    p = mm_group(i, n)
                    kb.op("act", lambda e: e.activation(out=sigb[s][:, (n - 7) * 512:(n - 6) * 512], in_=p[:], func=AF.Tanh, scale=0.5),
                          [p], [sigb[s]])
                for j in range(4):
                    kb.op("pe", lambda e: e.transpose(out=pQK[:, j * 128:(j + 1) * 128],
                                                      in_=qr[s][:].rearrange("p h d -> p (h d)")[:, j * 128:(j + 1) * 128],
                                                      identity=identb[:]), [qr[s], identb], [pQK])
                for j in range(4):
                    kb.op("pe", lambda e: e.transpose(out=pQK[:, 512 + j * 128:512 + (j + 1) * 128],
                                                      in_=kr[s][:].rearrange("p h d -> p (h d)")[:, j * 128:(j + 1) * 128],
                                                      identity=identb[:]), [kr[s], identb], [pQK])
                kb.op("dve", lambda e: e.tensor_copy(out=qkT[s][:], in_=pQK[:].rearrange("p (k t) -> p k t", k=8)), [pQK], [qkT[s]])
                kb.dma(QT_d.rearrange("(j p) s -> p j s", p=128)[:, :, i * 128:(i + 1) * 128], qkT[s][:, 0:4, :],
                       reads=[qkT[s]], writes=[dQT], sbuf=qkT[s])
                kb.dma(KT_d.rearrange("(j p) s -> p j s", p=128)[:, :, i * 128:(i + 1) * 128], qkT[s][:, 4:8, :],
                       reads=[qkT[s]], writes=[dKT], sbuf=qkT[s])
                for g in range(8):
                    kb.op("pe", lambda e: e.matmul(pZ[:, g * 64:(g + 1) * 64], lhsT=wmT[:, g, :], rhs=vh[s][:, g * 64:(g + 1) * 64],
                                                   start=True, stop=True), [wmT, vh[s]], [pZ])
                kb.op("dve", lambda e: e.tensor_tensor(out=z1[:], in0=pZ[:], in1=gng[:], op=ALU.mult), [pZ, gng], [z1])
                kb.op("dve", lambda e: e.tensor_tensor(out=z1[:].rearrange("p (g d) -> p g d", g=8),
                                                       in0=z1[:].rearrange("p (g d) -> p g d", g=8),
                                                       in1=bs[:].unsqueeze(2).to_broadcast([128, 8, 64]), op=ALU.add), [z1, bs], [z1])
                kb.op("dve", lambda e: e.tensor_tensor(out=sg[s][:], in0=z1[:], in1=u[s][:], op=ALU.mult), [z1, u[s]], [sg[s]])
            tail2(NT - 1)

            pk = pP[0]
            kT8 = kb.sb(pes, "kT8", [8, 128])
            km8 = kb.sb(pes, "km8", [8, 1])
            dg8 = kb.sb(pes, "dg8", [8, 8])
            ones8 = kb.sb(pes, "ones8", [8, 128])
            KM = kb.sb(pes, "KM", [128, 8])
            kb.op("pe", lambda e: e.transpose(out=pk[0:8, 0:128], in_=kmax2[:], identity=identf[:]), [kmax2, identf], [pk])
            kb.op("dve", lambda e: e.tensor_copy(out=kT8[:], in_=pk[0:8, 0:128]), [pk], [kT8])
            kb.op("dve", lambda e: e.tensor_reduce(out=km8[:], in_=kT8[:], axis=AX.X, op=ALU.max), [kT8], [km8])
            kb.op("dve", lambda e: e.tensor_single_scalar(out=dg8[:], in_=identf[0:8, 0:8], scalar=km8[:, 0:1], op=ALU.mult),
                  [identf, km8], [dg8])
            kb.op("dve", lambda e: e.memset(ones8[:], 1.0), [], [ones8])
            kb.op("pe", lambda e: e.matmul(pk[:, 256:264], lhsT=ones8[:], rhs=dg8[:], start=True, stop=True), [ones8, dg8], [pk])
            kb.op("dve", lambda e: e.tensor_copy(out=KM[:], in_=pk[:, 256:264]), [pk], [KM])
            kb.op("dve", lambda e: e.tensor_tensor(out=negshift[:], in0=qsq[:], in1=KM[:].unsqueeze(1).to_broadcast([128, NT, 8]),
                                                   op=ALU.mult), [qsq, KM], [negshift])
            kb.op("act", lambda e: e.activation(out=negshift[:], in_=negshift[:], func=AF.Sqrt, scale=1.0404), [negshift], [negshift])
            kb.op("dve", lambda e: e.tensor_single_scalar(out=negshift[:], in_=negshift[:], scalar=-1.0, op=ALU.mult),
                  [negshift], [negshift])
            kb.barrier()

    def phase2():
        conv = conv_gen()

        def conv_step(n=1):
            for _ in range(n):
                try:
                    next(conv)
                except StopIteration:
                    return

        with contextlib.ExitStack() as pes:
            alloc_stg(pes, True)
            KA = [kb.sb(pes, "KA%d" % i, [96, S], BF16) for i in range(2)]
            QA = [kb.sb(pes, "QA%d" % i, [96, S], BF16) for i in range(2)]
            VA = [kb.sb(pes, "VA%d" % i, [128, NT, 65], BF16) for i in range(2)]
            AO = [kb.sb(pes, "AO%d" % i, [128, NT, 64], BF16) for i in range(2)]
            pastneg = kb.sb(pes, "pastneg", [128, NT, 32])
            ownind = kb.sb(pes, "ownind", [128, NT, 32])
            km32 = kb.sb(pes, "km32", [64, NB])
            kmT = kb.sb(pes, "kmT", [64, 32], BF16)
            gm = kb.sb(pes, "gm", [128, 16, 32])
            m8 = kb.sb(pes, "m8", [128, 16, 8])
            sel = kb.sb(pes, "sel", [128, 16, 32])
            bias2 = kb.sb(pes, "bias2", [128, 16, 96])
            PT = [kb.sb(pes, "PT%d" % i, [128, 512], BF16) for i in range(5)]
            rz = kb.sb(pes, "rz", [128, 1])
            pSb = [kb.ps(pes, "pS%d" % i, [128, 512]) for i in range(5)]
            pG = kb.ps(pes, "pG", [128, 512])
            pO = [kb.ps(pes, "pO%d" % i, [128, 512]) for i in range(2)]
            kb.dma(pastneg[:], pastneg_d[:, :, :], writes=[pastneg], sbuf=pastneg)
            kb.dma(ownind[:], ownind_d[:, :, :], writes=[ownind], sbuf=ownind)
            kb.op("dve", lambda e: e.memset(bias2[:], 0.0), [], [bias2])
            kb.op("dve", lambda e: e.memset(kmT[:], 0.0), [], [kmT])
            for i in range(2):
                kb.dma(KA[i][64:96, :], ind_d[:, :], writes=[KA[i]], sbuf=KA[i])
            scale = 0.125
            CB = 16
            sctr = [0]
            def head_loads(h):
                s = h % 2
                kb.dma(KA[s][0:64, :], KT_d[h * 64:(h + 1) * 64, :], reads=[dKT], writes=[KA[s]], sbuf=KA[s])
                kb.dma(QA[s][0:64, :], QT_d[h * 64:(h + 1) * 64, :], reads=[dQT], writes=[QA[s]], sbuf=QA[s])
                kb.dma(VA[s][:], VS_d.rearrange("t p h c -> p t h c")[:, :, h, :], reads=[dVS], writes=[VA[s]], sbuf=VA[s])

            head_loads(0)
            for h in range(8):
                s = h % 2
                kb.op("dve", lambda e: e.tensor_reduce(out=km32[:], in_=KA[s][0:64, :].rearrange("p (n l) -> p n l", l=256),
                                                       axis=AX.X, op=ALU.add), [KA[s]], [km32])
                kb.op("act", lambda e: e.activation(out=kmT[:, 0:NB], in_=km32[:], func=AF.Copy, scale=1.0 / 256), [km32], [kmT])
                for c0 in range(0, NT, CB):
                    nb_ = min(CB, NT - c0)
                    for cc in range(nb_):
                        c = c0 + cc
                        kb.op("pe", lambda e: e.matmul(pG[:, cc * 32:(cc + 1) * 32], lhsT=QA[s][0:64, c * 128:(c + 1) * 128],
                                                       rhs=kmT[:, :], start=True, stop=True), [QA[s], kmT], [pG])
                    kb.op("dve", lambda e: e.tensor_tensor(out=gm[:, 0:nb_, :], in0=pG[:, 0:nb_ * 32].rearrange("p (c n) -> p c n", n=32),
                                                           in1=pastneg[:, c0:c0 + nb_, :], op=ALU.add), [pG, pastneg], [gm])
                    for cc in range(nb_):
                        kb.op("dve", lambda e: e.max(out=m8[:, cc, :], in_=gm[:, cc, :]), [gm], [m8])
                    kb.op("dve", lambda e: e.tensor_tensor(out=sel[:, 0:nb_, :], in0=gm[:, 0:nb_, :],
                                                           in1=m8[:, 0:nb_, 2:3].to_broadcast([128, nb_, 32]), op=ALU.is_ge),
                          [gm, m8], [sel])
                    kb.op("dve", lambda e: e.tensor_tensor(out=sel[:, 0:nb_, :], in0=sel[:, 0:nb_, :],
                                                           in1=ownind[:, c0:c0 + nb_, :], op=ALU.max), [sel, ownind], [sel])
                    kb.op("dve", lambda e: e.tensor_scalar(out=sel[:, 0:nb_, :], in0=sel[:, 0:nb_, :], scalar1=-NEGM, scalar2=NEGM,
                                                           op0=ALU.mult, op1=ALU.add), [sel], [sel])
                    kb.op("dve", lambda e: e.tensor_tensor(out=bias2[:, 0:nb_, 64:96], in0=sel[:, 0:nb_, :],
                                                           in1=negshift[:, c0:c0 + nb_, h:h + 1].to_broadcast([128, nb_, 32]),
                                                           op=ALU.add), [sel, negshift], [bias2])
                    for c4 in range(0, nb_, 4):
                        pb = pSb[(c4 // 4) % 4]
                        n4 = min(4, nb_ - c4)
                        for cc in range(c4, c4 + n4):
                            kb.op("pe", lambda e: e.transpose(out=pb[0:96, (cc - c4) * 128:(cc - c4 + 1) * 128], in_=bias2[:, cc, :],
                                                              identity=identf[:]), [bias2, identf], [pb])
                        kb.op("act", lambda e: e.activation(out=QA[s][64:96, (c0 + c4) * 128:(c0 + c4 + n4) * 128],
                                                            in_=pb[64:96, 0:n4 * 128], func=AF.Copy), [pb], [QA[s]])
                if h + 1 < 8:
                    head_loads(h + 1)
                groups = []
                for c in range(NT):
                    js = list(range(c + 1))
                    for g0 in range(0, len(js), 4):
                        groups.append((c, js[g0:g0 + 4]))

                def emit_qk(gi):
                    c, js = groups[gi]
                    pb = pSb[sctr[0] % 5]
                    sctr[0] += 1
                    for jj, j in enumerate(js):
                        kb.op("pe", lambda e: e.matmul(pb[:, jj * 128:(jj + 1) * 128], lhsT=KA[s][0:96, j * 128:(j + 1) * 128],
                                                       rhs=QA[s][0:96, c * 128:(c + 1) * 128], start=True, stop=True),
                              [KA[s], QA[s]], [pb])
                    return pb

                LA = 4
                pend = [emit_qk(g_) for g_ in range(min(LA, len(groups)))]
                for gi in range(len(groups)):
                    c, js = groups[gi]
                    pb = pend.pop(0)
                    if gi + LA < len(groups):
                        pend.append(emit_qk(gi + LA))
                    pt = PT[gi % 5]
                    n = len(js)
                    kb.op("act", lambda e: e.activation(out=pt[:, 0:n * 128], in_=pb[:, 0:n * 128], func=AF.Exp, scale=scale),
                          [pb], [pt])
                    if js[-1] == c:
                        kb.op("dve", lambda e: e.tensor_tensor(out=pt[:, (n - 1) * 128:n * 128], in0=pt[:, (n - 1) * 128:n * 128],
                                                               in1=trib[:], op=ALU.mult), [pt, trib], [pt])
                    po = pO[c % 2]
                    for jj, j in enumerate(js):
                        kb.op("pe", lambda e: e.matmul(po[:, 0:65], lhsT=pt[:, jj * 128:(jj + 1) * 128], rhs=VA[s][:, j, :],
                                                       start=(j == 0), stop=(j == c)), [pt, VA[s]], [po])
                    if js[-1] == c:
                        kb.op("dve", lambda e: e.reciprocal(out=rz[:], in_=po[:, 64:65]), [po], [rz])
                        kb.op("dve", lambda e: e.tensor_single_scalar(out=AO[s][:, c, :], in_=po[:, 0:64], scalar=rz[:, 0:1],
                                                                      op=ALU.mult), [po, rz], [AO[s]])
                        if c % 4 == 3:
                            conv_step(2 if NT >= 64 else 40)
                kb.dma(ATT_d.rearrange("(t p) c -> p t c", p=128)[:, :, h * 64:(h + 1) * 64], AO[s][:], reads=[AO[s]],
                       writes=[dATT], sbuf=AO[s])
            conv_step(10000)
            kb.barrier()

    def phase3a():
        with contextlib.ExitStack() as pes:
            alloc_stg(pes)
            mproj = kb.sb(pes, "mproj_sb", [128, 4, D], BF16)
            wout = kb.sb(pes, "wout_sb", [128, KC, D], BF16)
            xwq = kb.sb(pes, "xwq_sb", [128, KC, 512], BF16)
            xwkv = kb.sb(pes, "xwkv_sb", [128, KC, D], BF16)
            xwo = kb.sb(pes, "xwo_sb", [128, 4, D], BF16)
            load_w(mproj, mproj_d, 512, D, scale=0.5)
            load_w(wout, wout_d, D, D)
            load_w(xwq, xwq_d, D, 512)
            load_w(xwkv, xwkv_d, D, D)
            load_w(xwo, xwo_d, 512, D)
            kmT = kb.sb(pes, "xkmT", [128, 4, 256], BF16)
            vm = kb.sb(pes, "xvm", [128, 2, 512], BF16)
            mT = kb.sb(pes, "mT", [128, KC, 256], BF16)
            xs = [kb.sb(pes, "x3s%d" % i, [128, D]) for i in range(3)]
            att = [kb.sb(pes, "att%d" % i, [128, 512], BF16) for i in range(3)]
            sga = [kb.sb(pes, "sga%d" % i, [128, D], BF16) for i in range(3)]
            mbs = [kb.sb(pes, "mbs%d" % i, [128, D]) for i in range(3)]
            two = lambda name, shape, dt=F32: [kb.sb(pes, "%s_%d" % (name, i), shape, dt) for i in range(2)]
            attT_ = two("attT", [128, 4, 128], BF16)
            tmpf_ = two("tmpf", [128, D])
            mrg_ = two("mrg", [128, D], BF16)
            mrgT_ = two("mrgT", [128, KC, 128], BF16)
            x1_ = two("x1", [128, D])
            junk_ = two("junk3", [128, D], BF16)
            ssq_ = two("ssq3", [128, 1])
            rstd_ = two("rstd3", [128, 1])
            xh_ = two("xh3", [128, D], BF16)
            xT_ = two("xT3", [128, KC, 128], BF16)
            qxT_ = two("qxT", [128, 4, 128], BF16)
            mx_ = two("mx", [128, 4])
            rs_ = two("rs", [128, 4])
            Pb_ = two("Pb", [128, 4, 256], BF16)
            PbT_ = two("PbT", [128, 8, 128], BF16)
            ob_ = two("ob", [128, 512], BF16)
            oT_ = two("oT", [128, 4, 128], BF16)
            x2 = [kb.sb(pes, "x2o%d" % i, [128, D]) for i in range(2)]
            pTb = [kb.ps(pes, "p3T%d" % i, [128, 1024], BF16) for i in range(2)]
            pF = [kb.ps(pes, "p3F%d" % i, [128, 512]) for i in range(6)]
            fctr = [0]
            tctr = [0]

            def nf():
                p = pF[fctr[0] % 6]
                fctr[0] += 1
                return p

            def nt():
                p = pTb[tctr[0] % 2]
                tctr[0] += 1
                return p

            def norm_T(src, sl):
                junk, ssq, rstd, xh = junk_[sl], ssq_[sl], rstd_[sl], xh_[sl]
                kb.op("act", lambda e: e.activation(out=junk[:], in_=src[:], func=AF.Square, accum_out=ssq[:]), [src], [junk, ssq])
                kb.op("act", lambda e: e.activation(out=rstd[:], in_=ssq[:], func=AF.Sqrt, bias=EPS, scale=1.0 / D), [ssq], [rstd])
                kb.op("dve", lambda e: e.reciprocal(out=rstd[:], in_=rstd[:]), [rstd], [rstd])
                kb.op("act", lambda e: e.activation(out=xh[:], in_=src[:], func=AF.Copy, scale=rstd[:]), [src, rstd], [xh])

            def norm_T2(sl):
                xh = xh_[sl]
                p = nt()
                for kc in range(KC):
                    kb.op("pe", lambda e: e.transpose(out=p[:, kc * 128:(kc + 1) * 128], in_=xh[:, kc * 128:(kc + 1) * 128],
                                                      identity=identb[:]), [xh, identb], [p])
                return p

            for mt in range(2):
                kb.dma(xs[mt][:], mem_d[mt * 128:(mt + 1) * 128, :], reads=[DR], writes=[xs[mt]], sbuf=xs[mt])
                norm_T(xs[mt], mt)
                p = norm_T2(mt)
                kb.op("dve", lambda e: e.tensor_tensor(out=mT[:, :, mt * 128:(mt + 1) * 128], in0=p[:].rearrange("p (k t) -> p k t", k=KC),
                                                       in1=gains[:, 2, :].unsqueeze(2).to_broadcast([128, KC, 128]), op=ALU.mult),
                      [p, gains], [mT])
            for hh in range(4):
                p = nf()
                for kc in range(KC):
                    kb.op("pe", lambda e: e.matmul(p[:, 0:256], lhsT=xwkv[:, kc, hh * 128:(hh + 1) * 128], rhs=mT[:, kc, :],
                                                   start=(kc == 0), stop=(kc == KC - 1)), [xwkv, mT], [p])
                kb.op("act", lambda e: e.activation(out=kmT[:, hh, :], in_=p[:, 0:256], func=AF.Copy), [p], [kmT])
            for mt in range(2):
                p = nf()
                for kc in range(KC):
                    kb.op("pe", lambda e: e.matmul(p[:], lhsT=mT[:, kc, mt * 128:(mt + 1) * 128], rhs=xwkv[:, kc, 512:1024],
                                                   start=(kc == 0), stop=(kc == KC - 1)), [xwkv, mT], [p])
                kb.op("act", lambda e: e.activation(out=vm[:, mt, :], in_=p[:], func=AF.Copy), [p], [vm])

            def loads(i):
                s = i % 3
                r = slice(i * 128, (i + 1) * 128)
                kb.dma(att[s][:], ATT_d[r, :], reads=[dATT], writes=[att[s]], sbuf=att[s])
                kb.dma(sga[s][:], SIGA_d[r, :], reads=[dSIGA], writes=[sga[s]], sbuf=sga[s])
                kb.dma(mbs[s][:], MB_d[r, :], reads=[dMB], writes=[mbs[s]], sbuf=mbs[s])
                kb.dma(xs[s][:], x_d[r, :], reads=[DR], writes=[xs[s]], sbuf=xs[s])

            def tile_gen(i):
                s = i % 2
                l3 = i % 3
                attT, tmpf, mrg, mrgT, x1 = attT_[s], tmpf_[s], mrg_[s], mrgT_[s], x1_[s]
                xT, qxT, mx, rs, Pb, PbT, ob, oT = xT_[s], qxT_[s], mx_[s], rs_[s], Pb_[s], PbT_[s], ob_[s], oT_[s]
                if i + 1 < NT:
                    loads(i + 1)
                yield
                p = nt()
                for j in range(4):
                    kb.op("pe", lambda e: e.transpose(out=p[:, j * 128:(j + 1) * 128], in_=att[l3][:, j * 128:(j + 1) * 128],
                                                      identity=identb[:]), [att[l3], identb], [p])
                kb.op("act", lambda e: e.activation(out=attT[:], in_=p[:, 0:512].rearrange("p (k t) -> p k t", k=4), func=AF.Copy),
                      [p], [attT])
                yield
                for half in range(2):
                    hs = slice(half * 512, (half + 1) * 512)
                    p = nf()
                    for kc in range(4):
                        kb.op("pe", lambda e: e.matmul(p[:], lhsT=attT[:, kc, :], rhs=mproj[:, kc, hs], start=(kc == 0), stop=(kc == 3)),
                              [attT, mproj], [p])
                    kb.op("dve", lambda e: e.scalar_tensor_tensor(out=tmpf[:, hs], in0=sga[l3][:, hs], scalar=1.0, in1=p[:],
                                                                  op0=ALU.add, op1=ALU.mult), [p, sga[l3]], [tmpf])
                    kb.op("dve", lambda e: e.tensor_tensor(out=mrg[:, hs], in0=tmpf[:, hs], in1=mbs[l3][:, hs], op=ALU.add),
                          [tmpf, mbs[l3]], [mrg])
                yield
                p = nt()
                for kc in range(KC):
                    kb.op("pe", lambda e: e.transpose(out=p[:, kc * 128:(kc + 1) * 128], in_=mrg[:, kc * 128:(kc + 1) * 128],
                                                      identity=identb[:]), [mrg, identb], [p])
                kb.op("act", lambda e: e.activation(out=mrgT[:], in_=p[:].rearrange("p (k t) -> p k t", k=KC), func=AF.Copy), [p], [mrgT])
                yield
                for half in range(2):
                    hs = slice(half * 512, (half + 1) * 512)
                    p = nf()
                    for kc in range(KC):
                        kb.op("pe", lambda e: e.matmul(p[:], lhsT=mrgT[:, kc, :], rhs=wout[:, kc, hs], start=(kc == 0), stop=(kc == KC - 1)),
                              [mrgT, wout], [p])
                    kb.op("dve", lambda e: e.tensor_tensor(out=x1[:, hs], in0=p[:], in1=xs[l3][:, hs], op=ALU.add), [p, xs[l3]], [x1])
                norm_T(x1, s)
                yield
                p = norm_T2(s)
                kb.op("dve", lambda e: e.tensor_tensor(out=xT[:], in0=p[:].rearrange("p (k t) -> p k t", k=KC),
                                                       in1=gains[:, 1, :].unsqueeze(2).to_broadcast([128, KC, 128]), op=ALU.mult),
                      [p, gains], [xT])
                yield
                p = nf()
                for hh in range(4):
                    for kc in range(KC):
                        kb.op("pe", lambda e: e.matmul(p[:, hh * 128:(hh + 1) * 128], lhsT=xwq[:, kc, hh * 128:(hh + 1) * 128],
                                                       rhs=xT[:, kc, :], start=(kc == 0), stop=(kc == KC - 1)), [xwq, xT], [p])
                kb.op("act", lambda e: e.activation(out=qxT[:], in_=p[:].rearrange("p (k t) -> p k t", k=4), func=AF.Copy,
                                                    scale=128.0 ** -0.5), [p], [qxT])
                yield
                psc = [nf(), nf()]
                for hh in range(4):
                    pp = psc[hh // 2]
                    kb.op("pe", lambda e: e.matmul(pp[:, (hh % 2) * 256:(hh % 2 + 1) * 256], lhsT=qxT[:, hh, :], rhs=kmT[:, hh, :],
                                                   start=True, stop=True), [qxT, kmT], [pp])
                for half in range(2):
                    kb.op("dve", lambda e: e.tensor_reduce(out=mx[:, half * 2:half * 2 + 2],
                                                           in_=psc[half][:].rearrange("p (h m) -> p h m", h=2), axis=AX.X, op=ALU.max),
                          [psc[half]], [mx])
                kb.op("dve", lambda e: e.tensor_single_scalar(out=mx[:], in_=mx[:], scalar=-1.0, op=ALU.mult), [mx], [mx])
                for hh in range(4):
                    pp = psc[hh // 2]
                    kb.op("act", lambda e: e.activation(out=Pb[:, hh, :], in_=pp[:, (hh % 2) * 256:(hh % 2 + 1) * 256], func=AF.Exp,
                                                        bias=mx[:, hh:hh + 1], accum_out=rs[:, hh:hh + 1]), [pp, mx], [Pb, rs])
                yield
                p = nt()
                for hh in range(4):
                    for mt in range(2):
                        kb.op("pe", lambda e: e.transpose(out=p[:, (hh * 2 + mt) * 128:(hh * 2 + mt + 1) * 128],
                                                          in_=Pb[:, hh, mt * 128:(mt + 1) * 128], identity=identb[:]), [Pb, identb], [p])
                kb.op("act", lambda e: e.activation(out=PbT[:], in_=p[:].rearrange("p (k t) -> p k t", k=8), func=AF.Copy), [p], [PbT])
                yield
                p = nf()
                for hh in range(4):
                    for mt in range(2):
                        kb.op("pe", lambda e: e.matmul(p[:, hh * 128:(hh + 1) * 128], lhsT=PbT[:, hh * 2 + mt, :],
                                                       rhs=vm[:, mt, hh * 128:(hh + 1) * 128], start=(mt == 0), stop=(mt == 1)),
                              [PbT, vm], [p])
                kb.op("dve", lambda e: e.reciprocal(out=rs[:], in_=rs[:]), [rs], [rs])
                kb.op("dve", lambda e: e.tensor_tensor(out=ob[:].rearrange("p (h d) -> p h d", h=4),
                                                       in0=p[:].rearrange("p (h d) -> p h d", h=4),
                                                       in1=rs[:].unsqueeze(2).to_broadcast([128, 4, 128]), op=ALU.mult), [p, rs], [ob])
                yield
                p = nt()
                for j in range(4):
                    kb.op("pe", lambda e: e.transpose(out=p[:, j * 128:(j + 1) * 128], in_=ob[:, j * 128:(j + 1) * 128],
                                                      identity=identb[:]), [ob, identb], [p])
                kb.op("act", lambda e: e.activation(out=oT[:], in_=p[:, 0:512].rearrange("p (k t) -> p k t", k=4), func=AF.Copy), [p], [oT])
                yield
                for half in range(2):
                    hs = slice(half * 512, (half + 1) * 512)
                    p = nf()
                    for kc in range(4):
                        kb.op("pe", lambda e: e.matmul(p[:], lhsT=oT[:, kc, :], rhs=xwo[:, kc, hs], start=(kc == 0), stop=(kc == 3)),
                              [oT, xwo], [p])
                    kb.op("dve", lambda e: e.tensor_tensor(out=x2[s][:, hs], in0=p[:], in1=x1[:, hs], op=ALU.add), [p, x1], [x2[s]])
                kb.dma(X2_d[i * 128:(i + 1) * 128, :], x2[s][:], reads=[x2[s]], writes=[dX2], sbuf=x2[s])

            loads(0)
            active = []
            nxt = 0
            rounds = 0
            while nxt < NT or active:
                if nxt < NT and len(active) < 2 and (not active or rounds % 13 == 6 or len(active) == 0):
                    active.append(tile_gen(nxt))
                    nxt += 1
                for g_ in list(active):
                    try:
                        next(g_)
                    except StopIteration:
                        active.remove(g_)
                rounds += 1
            kb.barrier()

    def phase3b():
        WPQ_d = nc.dram_tensor("WPQ", [128, KC, 2048], BF16).ap()
        dWPQ = Buf("WPQ")
        with contextlib.ExitStack() as pes:
            skT = kb.sb(pes, "skT_sb", [128, 16, 128], BF16)
            iot = kb.sb(pes, "iot", [128, 128])
            fg = kb.sb(pes, "fg_sb", [128, D])
            with contextlib.ExitStack() as ies:
                alloc_stg(ies, True)
                for kc in range(KC):
                    k = kc % 2
                    kb.dma(stg[k][:], pwq_d[kc * 128:(kc + 1) * 128, :], writes=[stg[k]], sbuf=stg[k])
                    cast(cvo[k][:], stg[k][:], [stg[k]], [cvo[k]])
                    kb.dma(WPQ_d[:, kc, :], cvo[k][:], reads=[cvo[k]], writes=[dWPQ], sbuf=cvo[k])
                kb.dma(stg[0][:], skT_d.rearrange("p a n -> p (a n)"), writes=[stg[0]], sbuf=stg[0])
                kb.op("dve", lambda e: e.tensor_copy(out=skT[:].rearrange("p a n -> p (a n)"), in_=stg[0][:]), [stg[0]], [skT])
                kb.barrier()
            kb.dma(iot[:], iota_d[:, :], writes=[iot], sbuf=iot)
            kb.dma(fg[:], fg_d[:, :], writes=[fg], sbuf=fg)
            iob = kb.sb(pes, "iob", [128, 128], BF16)
            kb.op("dve", lambda e: e.tensor_copy(out=iob[:], in_=iot[:]), [iot], [iob])
            jTb = kb.sb(pes, "jTb", [128, 256], BF16)
            wq = [kb.sb(pes, "wq%d" % i, [128, KC, 128], BF16) for i in range(3)]
            x2s = [kb.sb(pes, "x2s%d" % i, [128, 2, D]) for i in range(2)]
            junk = kb.sb(pes, "junk4", [128, D], BF16)
            ssq = kb.sb(pes, "ssq4", [128, 1])
            rstd = kb.sb(pes, "rstd4", [128, 1])
            ssq2 = kb.sb(pes, "ssq5", [128, 1])
            rstd2 = kb.sb(pes, "rstd5", [128, 1])
            xh = kb.sb(pes, "xh4", [128, D])
            hT = [kb.sb(pes, "hT%d" % i, [128, KC, 256], BF16) for i in range(2)]
            pqT = kb.sb(pes, "pqT", [128, 16, 256], BF16)
            scs = [kb.sb(pes, "scq%d" % i, [128, 2048]) for i in range(2)]
            wk = kb.sb(pes, "wk", [128, 256])
            m16 = kb.sb(pes, "m16", [128, 16, 16])
            i16 = kb.sb(pes, "i16", [128, 16, 16], U32)
            i16f = kb.sb(pes, "i16f", [128, 16, 16])
            cand = kb.sb(pes, "cand", [128, 8, 256])
            t16 = kb.sb(pes, "t16", [128, 8, 16])
            pos = kb.sb(pes, "pos", [128, 8, 16], U32)
            pa = kb.sb(pes, "pa", [128, 8, 16], U32)
            pbb = kb.sb(pes, "pbb", [128, 8, 16], U32)
            paf = kb.sb(pes, "paf", [128, 8, 16])
            pbf = kb.sb(pes, "pbf", [128, 8, 16])
            jl = [kb.sb(pes, "jl%d" % i, [128, 3, 128]) for i in range(2)]
            et = kb.sb(pes, "et", [128, 8, 16])
            es_ = kb.sb(pes, "es_", [128, 8])
            jT = kb.sb(pes, "jT", [128, 3, 256])
            Ub = [kb.sb(pes, "Ub%d" % i, [128, 128], BF16) for i in range(4)]
            Vb4 = [kb.sb(pes, "Vb4%d" % i, [128, 4, 128], BF16) for i in range(2)]
            Gs = kb.sb(pes, "Gs", [128, 256, 128], BF16)
            wd = [kb.sb(pes, "wd%d" % i, [128, 2, KC, 128], BF16) for i in range(3)]
            wu = [kb.sb(pes, "wu%d" % i, [128, 2, D], BF16) for i in range(3)]
            ga = [kb.sb(pes, "ga%d" % i, [128, 256]) for i in range(3)]
            GA = [kb.sb(pes, "GA%d" % i, [128, 256], BF16) for i in range(3)]
            x3s = [kb.sb(pes, "x3_%d" % i, [128, D]) for i in range(2)]
            ssq2s = [kb.sb(pes, "ssq5_%d" % i, [128, 1]) for i in range(2)]
            rstd2s = [kb.sb(pes, "rstd5_%d" % i, [128, 1]) for i in range(2)]
            pacc = [kb.ps(pes, "pacc%d" % i, [128, 512]) for i in range(4)]
            pA = [kb.ps(pes, "pA%d" % i, [128, 512]) for i in range(2)]
            pX = [kb.ps(pes, "pX%d" % i, [128, 512]) for i in range(2)]
            xctr = [0]
            wqc = [0]

            def nx():
                p = pX[xctr[0] % 2]
                xctr[0] += 1
                return p

            def step(gen, n=1):
                if gen is None:
                    return
                for _ in range(n):
                    try:
                        next(gen)
                    except StopIteration:
                        return

            def prea_gen(st):
                sl = st % 2
                for sub in range(2):
                    r = slice(st * 256 + sub * 128, st * 256 + (sub + 1) * 128)
                    kb.dma(x2s[sl][:, sub, :], X2_d[r, :], reads=[dX2], writes=[x2s[sl]], sbuf=x2s[sl])
                    yield
                    kb.op("act", lambda e: e.activation(out=junk[:], in_=x2s[sl][:, sub, :], func=AF.Square, accum_out=ssq[:]),
                          [x2s[sl]], [junk, ssq])
                    kb.op("act", lambda e: e.activation(out=rstd[:], in_=ssq[:], func=AF.Sqrt, bias=EPS, scale=1.0 / D), [ssq], [rstd])
                    kb.op("dve", lambda e: e.reciprocal(out=rstd[:], in_=rstd[:]), [rstd], [rstd])
                    kb.op("act", lambda e: e.activation(out=xh[:], in_=x2s[sl][:, sub, :], func=AF.Copy, scale=rstd[:]),
                          [x2s[sl], rstd], [xh])
                    yield
                    for k2 in range(2):
                        p = nx()
                        for kk in range(4):
                            kc = k2 * 4 + kk
                            kb.op("pe", lambda e: e.transpose(out=p[:, kk * 128:(kk + 1) * 128], in_=xh[:, kc * 128:(kc + 1) * 128],
                                                              identity=identf[:]), [xh, identf], [p])
                        kb.op("dve", lambda e: e.tensor_tensor(out=hT[sl][:, k2 * 4:(k2 + 1) * 4, sub * 128:(sub + 1) * 128],
                                                               in0=p[:].rearrange("p (k t) -> p k t", k=4),
                                                               in1=gains[:, 3, k2 * 4:(k2 + 1) * 4].unsqueeze(2).to_broadcast([128, 4, 128]),
                                                               op=ALU.mult), [p, gains], [hT[sl]])
                        yield
                for hp in range(16):
                    w_ = wq[wqc[0] % 3]
                    wqc[0] += 1
                    kb.dma(w_[:], WPQ_d[:, :, hp * 128:(hp + 1) * 128], reads=[dWPQ], writes=[w_], sbuf=w_)
                    p = nx()
                    for kc in range(KC):
                        kb.op("pe", lambda e: e.matmul(p[:, 0:256], lhsT=w_[:, kc, :], rhs=hT[sl][:, kc, :],
                                                       start=(kc == 0), stop=(kc == KC - 1)), [w_, hT[sl]], [p])
                    kb.op("act", lambda e: e.activation(out=pqT[:, hp, :], in_=p[:, 0:256], func=AF.Copy), [p], [pqT])
                    yield
                for sub in range(2):
                    ts = slice(sub * 128, (sub + 1) * 128)
                    for q4 in range(4):
                        p = nx()
                        for k4 in range(4):
                            hp = q4 * 4 + k4
                            kb.op("pe", lambda e: e.matmul(p[:, k4 * 128:(k4 + 1) * 128], lhsT=pqT[:, hp, ts], rhs=skT[:, hp, :],
                                                           start=True, stop=True), [pqT, skT], [p])
                        kb.op("act", lambda e: e.activation(out=scs[sub][:, q4 * 512:(q4 + 1) * 512], in_=p[:], func=AF.Copy), [p], [scs[sub]])
                        yield

            def topk_gen(st):
                for sub in range(2):
                    scq = scs[sub]
                    jl_ = jl[sub]
                    for hp in range(16):
                        scv = scq[:, hp * 128:(hp + 1) * 128]
                        kb.op("dve", lambda e: e.max(out=m16[:, hp, 0:8], in_=scv), [scq], [m16])
                        kb.op("dve", lambda e: e.max_index(out=i16[:, hp, 0:8], in_max=m16[:, hp, 0:8], in_values=scv), [scq, m16], [i16])
                        yield
                        kb.op("dve", lambda e: e.match_replace(out=wk[:, 0:128], in_to_replace=m16[:, hp, 0:8], in_values=scv,
                                                               imm_value=-1e30), [scq, m16], [wk])
                        kb.op("dve", lambda e: e.max(out=m16[:, hp, 8:16], in_=wk[:, 0:128]), [wk], [m16])
                        kb.op("dve", lambda e: e.max_index(out=i16[:, hp, 8:16], in_max=m16[:, hp, 8:16], in_values=wk[:, 0:128]), [wk, m16], [i16])
                        yield
                    kb.op("dve", lambda e: e.tensor_copy(out=i16f[:], in_=i16[:]), [i16], [i16f])
                    m16v = m16[:].rearrange("p (h s) k -> p h s k", s=2)
                    i16v = i16f[:].rearrange("p (h s) k -> p h s k", s=2)
                    yield
                    kb.op("dve", lambda e: e.tensor_tensor(out=cand[:].rearrange("p h (a b) -> p h a b", b=16),
                                                           in0=m16v[:, :, 0, :].unsqueeze(3).to_broadcast([128, 8, 16, 16]),
                                                           in1=m16v[:, :, 1, :].unsqueeze(2).to_broadcast([128, 8, 16, 16]), op=ALU.add),
                          [m16], [cand])
                    yield
                    for h in range(8):
                        kb.op("dve", lambda e: e.max(out=t16[:, h, 0:8], in_=cand[:, h, :]), [cand], [t16])
                        kb.op("dve", lambda e: e.max_index(out=pos[:, h, 0:8], in_max=t16[:, h, 0:8], in_values=cand[:, h, :]), [cand, t16], [pos])
                        yield
                        kb.op("dve", lambda e: e.match_replace(out=wk[:], in_to_replace=t16[:, h, 0:8], in_values=cand[:, h, :],
                                                               imm_value=-1e30), [cand, t16], [wk])
                        kb.op("dve", lambda e: e.max(out=t16[:, h, 8:16], in_=wk[:]), [wk], [t16])
                        kb.op("dve", lambda e: e.max_index(out=pos[:, h, 8:16], in_max=t16[:, h, 8:16], in_values=wk[:]), [wk, t16], [pos])
                        yield
                    kb.op("dve", lambda e: e.tensor_tensor(out=et[:], in0=t16[:], in1=t16[:, :, 0:1].to_broadcast([128, 8, 16]),
                                                           op=ALU.subtract), [t16], [et])
                    kb.op("dve", lambda e: e.tensor_single_scalar(out=pa[:], in_=pos[:], scalar=4, op=ALU.logical_shift_right), [pos], [pa])
                    kb.op("dve", lambda e: e.tensor_single_scalar(out=pbb[:], in_=pos[:], scalar=15, op=ALU.bitwise_and), [pos], [pbb])
                    yield
                    kb.op("dve", lambda e: e.tensor_copy(out=paf[:], in_=pa[:]), [pa], [paf])
                    kb.op("dve", lambda e: e.tensor_copy(out=pbf[:], in_=pbb[:]), [pbb], [pbf])
                    yield
                    kb.op("act", lambda e: e.activation(out=et[:], in_=et[:], func=AF.Exp), [et], [et])
                    eqv = scq[:].rearrange("p (h k a) -> p h k a", h=8, k=16)
                    io16 = iot[:, 0:16].unsqueeze(1).unsqueeze(1).to_broadcast([128, 8, 16, 16])
                    for side, pf in ((0, paf), (1, pbf)):
                        kb.op("dve", lambda e: e.tensor_tensor(out=eqv, in0=pf[:].unsqueeze(3).to_broadcast([128, 8, 16, 16]), in1=io16,
                                                               op=ALU.is_equal), [pf, iot], [scq])
                        yield
                        kb.op("dve", lambda e: e.tensor_tensor(out=eqv, in0=eqv,
                                                               in1=i16v[:, :, side, :].unsqueeze(2).to_broadcast([128, 8, 16, 16]),
                                                               op=ALU.mult), [scq, i16f], [scq])
                        yield
                        kb.op("dve", lambda e: e.tensor_reduce(out=jl_[:, side, :].rearrange("p (h k) -> p h k", h=8), in_=eqv,
                                                               axis=AX.X, op=ALU.add), [scq], [jl_])
                        yield
                    kb.op("dve", lambda e: e.tensor_reduce(out=es_[:], in_=et[:], axis=AX.X, op=ALU.add), [et], [es_])
                    kb.op("dve", lambda e: e.reciprocal(out=es_[:], in_=es_[:]), [es_], [es_])
                    kb.op("dve", lambda e: e.tensor_tensor(out=jl_[:, 2, :].rearrange("p (h k) -> p h k", h=8), in0=et[:],
                                                           in1=es_[:].unsqueeze(2).to_broadcast([128, 8, 16]), op=ALU.mult), [et, es_], [jl_])
                    yield

            def jt_emit():
                for sub in range(2):
                    ts = slice(sub * 128, (sub + 1) * 128)
                    p = nx()
                    for a in range(3):
                        kb.op("pe", lambda e: e.transpose(out=p[:, a * 128:(a + 1) * 128], in_=jl[sub][:, a, :], identity=identf[:]),
                              [jl[sub], identf], [p])
                    kb.op("act", lambda e: e.activation(out=jT[:, :, ts], in_=p[:, 0:384].rearrange("p (a t) -> p a t", a=3), func=AF.Copy),
                          [p], [jT])
                    kb.op("act", lambda e: e.activation(out=jTb[:, ts], in_=p[:, 128:256], func=AF.Copy), [p], [jTb])

            def ld(g):
                k = g % 3
                kb.dma(wd[k][:].rearrange("p a b c -> p (a b c)"), WD_d[g], reads=[dWD[g]], writes=[wd[k]], sbuf=wd[k])
                kb.dma(wu[k][:].rearrange("p a d -> p (a d)"), WU_d[g], reads=[dWU[g]], writes=[wu[k]], sbuf=wu[k])

            pending_post = []

            def post_b(st_):
                for sub in range(2):
                    x3 = x3s[sub]
                    kb.op("dve", lambda e: e.reciprocal(out=rstd2s[sub][:], in_=rstd2s[sub][:]), [rstd2s[sub]], [rstd2s[sub]])
                    kb.op("dve", lambda e: e.scalar_tensor_tensor(out=x3[:], in0=x3[:], scalar=rstd2s[sub][:, 0:1], in1=fg[:], op0=ALU.mult,
                                                                  op1=ALU.mult), [x3, rstd2s[sub], fg], [x3])
                    r = slice(st_ * 256 + sub * 128, st_ * 256 + (sub + 1) * 128)
                    kb.dma(out_d[r, :], x3[:], reads=[x3], writes=[], sbuf=x3)

            step(prea_gen(0), 100000)
            step(topk_gen(0), 100000)
            jt_emit()
            for st in range(NST):
                sl = st % 2
                hT_ = hT[sl]
                prea = prea_gen(st + 1) if st + 1 < NST else None
                topk = topk_gen(st + 1) if st + 1 < NST else None
                ld(0)
                ld(1)
                for t0 in range(0, 256, 4):
                    p = nx()
                    for tt in range(4):
                        t = t0 + tt
                        ub = Ub[t % 4]
                        if tt == 0:
                            vb4 = Vb4[(t0 // 4) % 2]
                            kb.op("dve", lambda e: e.tensor_tensor(out=vb4[:], in0=iob[:].unsqueeze(1).to_broadcast([128, 4, 128]),
                                                                   in1=jTb[:, t0:t0 + 4].unsqueeze(2).to_broadcast([128, 4, 128]),
                                                                   op=ALU.is_equal), [iob, jTb], [vb4])
                        kb.op("dve", lambda e: e.tensor_scalar(out=ub[:], in0=iob[:], scalar1=jT[:, 0, t:t + 1], scalar2=jT[:, 2, t:t + 1],
                                                               op0=ALU.is_equal, op1=ALU.mult), [iob, jT], [ub])
                        kb.op("pe", lambda e: e.matmul(p[:, tt * 128:(tt + 1) * 128], lhsT=ub[:], rhs=vb4[:, tt, :], start=True, stop=True),
                              [ub, vb4], [p])
                    kb.op("act", lambda e: e.activation(out=Gs[:, t0:t0 + 4, :], in_=p[:].rearrange("p (t n) -> p t n", t=4), func=AF.Copy),
                          [p], [Gs])
                    step(prea, 1)
                    if t0 == 16 and pending_post:
                        post_b(pending_post.pop(0))
                step(prea, 100000)

                def down(n2):
                    g, q = divmod(n2, 2)
                    k = g % 3
                    pa_ = pA[n2 % 2]
                    for kc in range(KC):
                        kb.op("pe", lambda e: e.matmul(pa_[:, 0:256], lhsT=wd[k][:, q, kc, :], rhs=hT_[:, kc, :], start=(kc == 0),
                                                       stop=(kc == KC - 1)), [wd[k], hT_], [pa_])

                down(0)
                for n2 in range(128):
                    g, q = divmod(n2, 2)
                    k = g % 3
                    if q == 0 and g + 2 < 64:
                        ld(g + 2)
                    if n2 + 1 < 128:
                        down(n2 + 1)
                    pa_ = pA[n2 % 2]
                    gk = ga[n2 % 3]
                    Gk = GA[n2 % 3]
                    kb.op("act", lambda e: e.activation(out=gk[:], in_=pa_[:, 0:256], func=GELU), [pa_], [gk])
                    kb.op("dve", lambda e: e.tensor_tensor(out=Gk[:], in0=gk[:], in1=Gs[:, :, n2], op=ALU.mult), [gk, Gs], [Gk])
                    for sub in range(2):
                        for half in range(2):
                            kb.op("pe", lambda e: e.matmul(pacc[sub * 2 + half][:], lhsT=Gk[:, sub * 128:(sub + 1) * 128],
                                                           rhs=wu[k][:, q, half * 512:(half + 1) * 512], start=(n2 == 0), stop=(n2 == 127)),
                                  [Gk, wu[k]], [pacc[sub * 2 + half]])
                    step(topk, 1)
                step(topk, 100000)
                if st + 1 < NST:
                    jt_emit()
                for sub in range(2):
                    x3 = x3s[sub]
                    for half in range(2):
                        hs = slice(half * 512, (half + 1) * 512)
                        kb.op("dve", lambda e: e.tensor_tensor(out=x3[:, hs], in0=pacc[sub * 2 + half][:], in1=x2s[sl][:, sub, hs], op=ALU.add),
                              [pacc[sub * 2 + half], x2s[sl]], [x3])
                    kb.op("act", lambda e: e.activation(out=junk[:], in_=x3[:], func=AF.Square, accum_out=ssq2s[sub][:]), [x3], [junk, ssq2s[sub]])
                for sub in range(2):
                    kb.op("act", lambda e: e.activation(out=rstd2s[sub][:], in_=ssq2s[sub][:], func=AF.Sqrt, bias=EPS, scale=1.0 / D),
                          [ssq2s[sub]], [rstd2s[sub]])
                pending_post.append(st)
            post_b(pending_post.pop(0))
            kb.barrier()

    if stop_after >= 1:
        phase1()
    if stop_after >= 2:
        phase2()
    if stop_after >= 3:
        phase3a()
    if stop_after >= 4:
        phase3b()
    kb.barrier(("sp",))
    es.close()
    return nc, kb


def host_consts(S):
    NT = S // 128
    ident = np.eye(128, dtype=np.float32)
    k = np.arange(128)
    tri = (k[:, None] <= k[None, :]).astype(np.float32)
    iota = np.tile(np.arange(128, dtype=np.float32)[None, :], (128, 1))
    half = 32
    freqs = (10000.0 ** (-np.arange(half, dtype=np.float32) / half)).astype(np.float32)
    pos = np.arange(S, dtype=np.float32)
    ang = (pos[:, None] * freqs[None, :]).astype(np.float32)
    cos = np.cos(ang).astype(np.float32).reshape(NT, 128, 32).transpose(1, 0, 2)
    sin = np.sin(ang).astype(np.float32).reshape(NT, 128, 32).transpose(1, 0, 2)
    ind = np.zeros((32, S), dtype=np.float32)
    for n in range(S // 256):
        ind[n, n * 256:(n + 1) * 256] = 1.0
    pastneg = np.zeros((NT, 32), dtype=np.float32)
    ownind = np.zeros((NT, 32), dtype=np.float32)
    for c in range(NT):
        own = c // 2
        pastneg[c, own:] = -1e30
        ownind[c, own] = 1.0
    return {
        "ident": ident, "tri": tri, "iota": iota,
        "cos": np.ascontiguousarray(cos), "sin": np.ascontiguousarray(sin),
        "ind": ind.astype(ml_dtypes.bfloat16),
        "pastneg": np.ascontiguousarray(np.broadcast_to(pastneg[None], (128, NT, 32))),
        "ownind": np.ascontiguousarray(np.broadcast_to(ownind[None], (128, NT, 32))),
    }


def host_weights(norm_mix_g, w_in, moba_w_proj, gmlp_norm_g, gmlp_w_s, gmlp_b_s, gmlp_w_proj, w_out, norm_xa_g,
                 norm_mem_g, xa_w_q, xa_w_kv, xa_w_o, norm_ffn_g, peer_w_q, peer_subkeys, peer_w_down, peer_w_up, final_g):
    f = lambda a: np.ascontiguousarray(np.asarray(a, dtype=np.float32))
    gains = np.stack([np.asarray(g, np.float32)[0].reshape(8, 128).T for g in (norm_mix_g, norm_xa_g, norm_mem_g, norm_ffn_g)], axis=1)
    wd = np.asarray(peer_w_down, np.float32)[0].reshape(128, 128, 8, 128)
    wdT = np.ascontiguousarray(wd.transpose(3, 1, 2, 0)).reshape(128, 131072)
    wu = np.asarray(peer_w_up, np.float32)[0].reshape(128, 131072)
    sk = np.asarray(peer_subkeys, np.float32)[0].reshape(16, 128, 128)
    return {
        "w_in": f(w_in[0]), "gproj": f(gmlp_w_proj[0]), "mproj": f(moba_w_proj[0]), "w_out": f(w_out[0]),
        "xa_wq": f(xa_w_q[0]), "xa_wkv": f(xa_w_kv[0]), "xa_wo": f(xa_w_o[0]), "peer_wq": f(peer_w_q[0]),
        "gains": f(gains),
        "gng": f(np.broadcast_to(np.asarray(gmlp_norm_g, np.float32)[0][None, :], (128, 512))),
        "bs": f(np.asarray(gmlp_b_s, np.float32)[0].T),
        "fg": f(np.broadcast_to(np.asarray(final_g, np.float32)[None, :], (128, D))),
        "wmT": f(np.asarray(gmlp_w_s, np.float32)[0].transpose(2, 0, 1)),
        "skT": f(sk.transpose(2, 0, 1)),
        "wdT": wdT, "wu": f(wu),
    }


_CACHE = {}


def kernel(x, mem, **w):
    x = np.asarray(x, np.float32)
    mem = np.asarray(mem, np.float32)
    B, S, _ = x.shape
    if S not in _CACHE:
        _CACHE[S] = build(S)[0]
    nc = _CACHE[S]
    shared = dict(host_consts(S))
    shared.update(host_weights(**w))
    in_maps = []
    for b in range(B):
        m = dict(shared)
        m["x"] = np.ascontiguousarray(x[b])
        m["mem"] = np.ascontiguousarray(mem[b])
        in_maps.append(m)
    res = run_bass_kernel_spmd(nc, in_maps, core_ids=list(range(B)))
    return np.stack([np.asarray(r["out"], np.float32) for r in res.results], axis=0)
```
